# Optimizing a Trainium2 kernel written in Bass

```python
import jax
import jax.numpy as jnp
from jax import lax
import numpy as np


D_MODEL = 1024
BATCH = 4
SEQ = 8192
DEPTH = 1

GRID_W = 64
MEM_LEN = 256
NA_HEADS = 8
NA_HEAD_DIM = 64
NA_WIN_ROWS = 8
NA_WIN_COLS = 16
FT_GROUPS = 4
FT_GROUP_DIM = 128
MEM_HEADS = 4
MEM_HEAD_DIM = 128
NA_WIDTH = NA_HEADS * NA_HEAD_DIM
FT_WIDTH = FT_GROUPS * FT_GROUP_DIM
MEM_WIDTH = MEM_HEADS * MEM_HEAD_DIM
MIX_WIDTH = NA_WIDTH + FT_WIDTH + MEM_WIDTH
IN_WIDTH = 3 * NA_WIDTH + FT_WIDTH + MEM_WIDTH
N_EXPERTS = 32
TOP_K = 4
D_EXPERT = D_MODEL
SWIGLU_LIMIT = 7.0
SWIGLU_ALPHA = 1.702
MOE_BLOCK = 512
EPS = 1e-6

kernel_name = 'hymba_natten_fnet_memory_moe_encoder'


def rmsnorm(x, g):
    xf = x.astype(jnp.float32)
    y = xf * lax.rsqrt(jnp.mean(xf * xf, axis=-1, keepdims=True) + EPS)
    return (y * g.astype(jnp.float32)).astype(x.dtype)


def neighbourhood_attention(q, k, v, rel_bias):
    b, s = q.shape[0], q.shape[1]
    rows = s // GRID_W
    win_r = min(NA_WIN_ROWS, rows)

    def to_grid(t):
        return t.reshape(b, rows, GRID_W, NA_HEADS, NA_HEAD_DIM).transpose(0, 3, 1, 2, 4)

    qg, kg, vg = to_grid(q), to_grid(k), to_grid(v)
    r = np.arange(rows)
    row_start = np.clip(r - win_r // 2, 0, rows - win_r)
    row_idx = row_start[:, None] + np.arange(win_r)[None, :]
    k_rows = kg[:, :, row_idx]
    v_rows = vg[:, :, row_idx]
    c = np.arange(GRID_W)
    col_start = np.clip(c - NA_WIN_COLS // 2, 0, GRID_W - NA_WIN_COLS)
    col_in = (c[None, :] >= col_start[:, None]) & (c[None, :] < col_start[:, None] + NA_WIN_COLS)
    dr = row_idx - r[:, None]
    dc = np.clip(c[None, :] - c[:, None], -(NA_WIN_COLS - 1), NA_WIN_COLS - 1)
    bias = rel_bias[:, dr[:, None, :, None] + (NA_WIN_ROWS - 1), dc[None, :, None, :] + (NA_WIN_COLS - 1)]
    scores = jnp.einsum('bhrqd,bhrjcd->bhrqjc', qg, k_rows).astype(jnp.float32) * (NA_HEAD_DIM ** -0.5)
    scores = jnp.where(col_in[None, None, None, :, None, :], scores + bias[None].astype(jnp.float32), -jnp.inf)
    p = jax.nn.softmax(scores.reshape(b, NA_HEADS, rows, GRID_W, win_r * GRID_W), axis=-1).astype(v.dtype)
    o = jnp.einsum('bhrqk,bhrkd->bhrqd', p, v_rows.reshape(b, NA_HEADS, rows, win_r * GRID_W, NA_HEAD_DIM))
    return o.transpose(0, 2, 3, 1, 4).reshape(b, s, NA_WIDTH)


def fourier_mix(u):
    b, s = u.shape[0], u.shape[1]
    ug = u.astype(jnp.float32).reshape(b, s, FT_GROUPS, FT_GROUP_DIM)
    f = jnp.fft.fft2(ug, axes=(1, 3), norm='ortho')
    return jnp.real(f).reshape(b, s, FT_WIDTH).astype(u.dtype)


def memory_attention(q, mem_n, w_mem_kv):
    b, s = q.shape[0], q.shape[1]
    kv = mem_n @ w_mem_kv
    k = kv[..., :MEM_WIDTH].reshape(b, -1, MEM_HEADS, MEM_HEAD_DIM)
    v = kv[..., MEM_WIDTH:].reshape(b, -1, MEM_HEADS, MEM_HEAD_DIM)
    qh = q.reshape(b, s, MEM_HEADS, MEM_HEAD_DIM)
    scores = jnp.einsum('bshd,bmhd->bhsm', qh, k).astype(jnp.float32) * (MEM_HEAD_DIM ** -0.5)
    p = jax.nn.softmax(scores, axis=-1).astype(v.dtype)
    return jnp.einsum('bhsm,bmhd->bshd', p, v).reshape(b, s, MEM_WIDTH)


def routed_ffn(h, router_w, router_b, w_gu, b_gu, w_down, b_down):
    b, s, d = h.shape
    t = b * s
    hf = h.reshape(t, d)
    logits = (hf @ router_w + router_b).astype(jnp.float32)
    top_val, top_idx = lax.top_k(logits, TOP_K)
    gates = jax.nn.softmax(top_val, axis=-1).astype(h.dtype)
    n_assign = t * TOP_K
    e_flat = top_idx.reshape(-1)
    tok_flat = jnp.repeat(jnp.arange(t, dtype=jnp.int32), TOP_K)
    order = jnp.argsort(e_flat, stable=True)
    e_sorted = e_flat[order]
    tok_sorted = tok_flat[order]
    gate_sorted = gates.reshape(-1)[order]
    counts = jnp.bincount(e_flat, length=N_EXPERTS)
    padded = (counts + MOE_BLOCK - 1) // MOE_BLOCK * MOE_BLOCK
    padded_end = jnp.cumsum(padded)
    group_start = jnp.cumsum(counts) - counts
    dest = (padded_end - padded)[e_sorted] + jnp.arange(n_assign, dtype=jnp.int32) - group_start[e_sorted]
    n_blocks = -(-n_assign // MOE_BLOCK) + N_EXPERTS
    n_slots = n_blocks * MOE_BLOCK
    slot_tok = jnp.zeros((n_slots,), jnp.int32).at[dest].set(tok_sorted)
    slot_gate = jnp.zeros((n_slots,), h.dtype).at[dest].set(gate_sorted)
    block_expert = jnp.minimum(
        jnp.searchsorted(padded_end, jnp.arange(n_blocks, dtype=jnp.int32) * MOE_BLOCK, side='right'),
        N_EXPERTS - 1)

    def expert_block(args):
        tok_b, gate_b, e = args
        xb = hf[tok_b]
        gu = xb @ w_gu[e] + b_gu[e]
        x_glu = jnp.minimum(gu[:, :D_EXPERT], SWIGLU_LIMIT)
        x_lin = jnp.clip(gu[:, D_EXPERT:], -SWIGLU_LIMIT, SWIGLU_LIMIT)
        act = x_glu * jax.nn.sigmoid(SWIGLU_ALPHA * x_glu) * (x_lin + 1.0)
        y = act @ w_down[e] + b_down[e]
        return y * gate_b[:, None]

    y = lax.map(expert_block, (slot_tok.reshape(n_blocks, MOE_BLOCK), slot_gate.reshape(n_blocks, MOE_BLOCK), block_expert))
    out = jax.ops.segment_sum(y.reshape(n_slots, d), slot_tok, num_segments=t)
    return out.reshape(b, s, d)


def setup_inputs(seed: int = 0) -> dict:
    key = jax.random.key(seed)
    ks = jax.random.split(key, 18)

    def nrm(k, shape, scale):
        return jax.random.normal(k, shape, jnp.float32) * scale

    return {
        'x': nrm(ks[0], (BATCH, SEQ, D_MODEL), 1.0),
        'mem': nrm(ks[1], (BATCH, MEM_LEN, D_MODEL), 1.0),
        'g_mix': 1.0 + nrm(ks[2], (DEPTH, D_MODEL), 0.02),
        'g_mem': 1.0 + nrm(ks[3], (DEPTH, D_MODEL), 0.02),
        'w_in': nrm(ks[4], (DEPTH, D_MODEL, IN_WIDTH), D_MODEL ** -0.5),
        'w_mem_kv': nrm(ks[5], (DEPTH, D_MODEL, 2 * MEM_WIDTH), D_MODEL ** -0.5),
        'na_rel_bias': nrm(ks[6], (DEPTH, NA_HEADS, 2 * NA_WIN_ROWS - 1, 2 * NA_WIN_COLS - 1), 0.1),
        'g_grp': 1.0 + nrm(ks[7], (DEPTH, MIX_WIDTH), 0.02),
        'w_out': nrm(ks[8], (DEPTH, MIX_WIDTH, D_MODEL), MIX_WIDTH ** -0.5),
        'g_ffn': 1.0 + nrm(ks[9], (DEPTH, D_MODEL), 0.02),
        'router_w': nrm(ks[10], (DEPTH, D_MODEL, N_EXPERTS), D_MODEL ** -0.5),
        'router_b': nrm(ks[11], (DEPTH, N_EXPERTS), 0.01),
        'w_gu': nrm(ks[12], (DEPTH, N_EXPERTS, D_MODEL, 2 * D_EXPERT), D_MODEL ** -0.5),
        'b_gu': nrm(ks[13], (DEPTH, N_EXPERTS, 2 * D_EXPERT), 0.01),
        'w_down': nrm(ks[14], (DEPTH, N_EXPERTS, D_EXPERT, D_MODEL), D_EXPERT ** -0.5),
        'b_down': nrm(ks[15], (DEPTH, N_EXPERTS, D_MODEL), 0.01),
        'g_final': 1.0 + nrm(ks[16], (D_MODEL,), 0.02),
    }


def reference(x, mem, g_mix, g_mem, w_in, w_mem_kv, na_rel_bias, g_grp, w_out, g_ffn,
              router_w, router_b, w_gu, b_gu, w_down, b_down, g_final):
    splits = [NA_WIDTH, 2 * NA_WIDTH, 3 * NA_WIDTH, 3 * NA_WIDTH + FT_WIDTH]
    for l in range(DEPTH):
        h = rmsnorm(x, g_mix[l])
        proj = h @ w_in[l]
        q_na, k_na, v_na, u_ft, q_mem = jnp.split(proj, splits, axis=-1)
        mem_n = rmsnorm(mem, g_mem[l])
        y_na = neighbourhood_attention(q_na, k_na, v_na, na_rel_bias[l])
        y_ft = fourier_mix(u_ft)
        y_mem = memory_attention(q_mem, mem_n, w_mem_kv[l])
        g = g_grp[l]
        y = jnp.concatenate([
            rmsnorm(y_na, g[:NA_WIDTH]),
            rmsnorm(y_ft, g[NA_WIDTH:NA_WIDTH + FT_WIDTH]),
            rmsnorm(y_mem, g[NA_WIDTH + FT_WIDTH:]),
        ], axis=-1)
        x = x + y @ w_out[l]
        h2 = rmsnorm(x, g_ffn[l])
        x = x + routed_ffn(h2, router_w[l], router_b[l], w_gu[l], b_gu[l], w_down[l], b_down[l])
    return rmsnorm(x, g_final)
```

```python
import os
import numpy as np
import ml_dtypes
import concourse.bass as bass
import concourse.mybir as mybir
from concourse.bass_utils import run_bass_kernel_spmd
from contextlib import ExitStack

F32 = mybir.dt.float32; BF16 = mybir.dt.bfloat16; U32 = mybir.dt.uint32
AF = mybir.ActivationFunctionType; ALU = mybir.AluOpType

NCORES = 8
D = 1024
SEQ = 8192
TOK = 4096
KVROWS = 72
KVTOK = KVROWS * 64
NEXP = 32
BLK = 512
HORD = [0, 2, 1, 3, 4, 6, 5, 7]
NBLK = 63
DEBUG = bool(int(os.environ.get("MK_DEBUG", "0")))
CUT = int(os.environ.get("MK_CUT", "0"))


class Dep:
    __slots__ = ("w", "r", "prev", "psum")
    def __init__(self, psum=False):
        self.w = []; self.r = []; self.prev = []; self.psum = psum


class Prog:
    NCH = {"sp": 3, "pool": 5}

    def __init__(self, nc, stack):
        self.nc = nc; self.stack = stack
        self.eng = {"pe": nc.tensor, "act": nc.scalar, "dve": nc.vector, "pool": nc.gpsimd, "sp": nc.sync}
        self.sem = {e: stack.enter_context(nc.semaphore("s_" + e)) for e in self.eng}
        self.count = {e: 0 for e in self.eng}
        self.waited = {e: {} for e in self.eng}
        self.chan = {}
        self.rr = {}
        self.ninst = 0

    def _sem(self, key):
        return self.sem[key] if key in self.sem else self.chan[key][0]

    def _wait(self, eng, events):
        best = {}
        for (k, v) in events:
            if k == eng and eng == "pe":
                continue
            if best.get(k, 0) < v:
                best[k] = v
        wd = self.waited[eng]
        for k, v in best.items():
            if wd.get(k, 0) >= v:
                continue
            wd[k] = v
            self.eng[eng].wait_ge(self._sem(k), v)

    @staticmethod
    def _collect(reads, writes, adds, eng=None):
        ev = []
        for d in reads:
            ev += d.w
            if d.psum:
                ev += [x for x in d.r if x[0] != eng]
        for d in writes:
            ev += d.w; ev += d.r
        for d in adds:
            ev += d.r; ev += d.prev
        return ev

    @staticmethod
    def _update(me, reads, writes, adds):
        for d in reads:
            d.r.append(me)
            if len(d.r) > 64:
                d.r = Prog._compress(d.r)
        for d in writes:
            d.prev = Prog._compress(d.w + d.r)
            d.w = [me]; d.r = []
        for d in adds:
            d.w.append(me)
            if len(d.w) > 64:
                d.w = Prog._compress(d.w)

    @staticmethod
    def _compress(evs):
        best = {}
        for k, v in evs:
            if best.get(k, 0) < v:
                best[k] = v
        return list(best.items())

    def op(self, eng, fn, reads=(), writes=(), adds=()):
        self._wait(eng, self._collect(reads, writes, adds, eng))
        self.count[eng] += 1
        fn(self.eng[eng]).then_inc(self.sem[eng], 1)
        self._update((eng, self.count[eng]), reads, writes, adds)
        self.ninst += 1

    def dma(self, q, fn, reads=(), writes=(), adds=()):
        self.rr[q] = (self.rr.get(q, -1) + 1) % self.NCH[q]
        chan = "%s%d" % (q, self.rr[q])
        if chan not in self.chan:
            self.chan[chan] = [self.stack.enter_context(self.nc.semaphore("c_" + chan)), 0]
        c = self.chan[chan]
        ev = self._collect(reads, writes, adds)
        if c[1] > 0:
            ev.append((chan, 16 * c[1]))
        self._wait(q, ev)
        c[1] += 1
        fn(self.eng[q]).then_inc(c[0], 16)
        self._update((chan, 16 * c[1]), reads, writes, adds)
        self.ninst += 1

    def barrier(self):
        ev = [(e, n) for e, n in self.count.items() if n > 0]
        ev += [(ch, 16 * c[1]) for ch, c in self.chan.items() if c[1] > 0]
        for e in self.eng:
            self._wait(e, ev)

    def drain(self, eng="sp"):
        ev = [(e, n) for e, n in self.count.items() if n > 0]
        ev += [(ch, 16 * c[1]) for ch, c in self.chan.items() if c[1] > 0]
        self._wait(eng, ev)


class Ring:
    def __init__(self, items, psum=False):
        self.items = items; self.deps = [Dep(psum) for _ in items]; self.i = -1
    def next(self):
        self.i = (self.i + 1) % len(self.items)
        return self.items[self.i], self.deps[self.i]


class Ctx:
    pass


def _alt(K):
    K.alt ^= 1
    return "act" if K.alt else "dve"


def copy_op(K, eng, out, in_, reads, writes=(), adds=()):
    if eng == "act":
        K.P.op("act", lambda e: e.copy(out, in_), reads=reads, writes=writes, adds=adds)
    else:
        K.P.op(eng, lambda e: e.tensor_copy(out, in_), reads=reads, writes=writes, adds=adds)


def rstd_ops(K, stat, dstat, n, width):
    P = K.P
    P.op("dve", lambda e: e.tensor_scalar(out=stat[:, n:2 * n], in0=stat[:, 0:n], scalar1=1.0 / width, scalar2=1e-6,
                                          op0=ALU.mult, op1=ALU.add), reads=[dstat], writes=[dstat])
    P.op("act", lambda e: e.sqrt(stat[:, n:2 * n], stat[:, n:2 * n]), reads=[dstat], writes=[dstat])
    P.op("dve", lambda e: e.reciprocal(stat[:, n:2 * n], stat[:, n:2 * n]), reads=[dstat], writes=[dstat])


def norm_tiles(K, srcs, dsrc, outb, doutb, g_ap, dg, width, np_=128):
    P = K.P
    n = len(srcs)
    stat, dstat = K.stat.next()
    for t, s in enumerate(srcs):
        P.op("act", lambda e, s=s, t=t: e.activation(out=K.junk[0:np_, 0:width], in_=s, func=AF.Square,
                                                      accum_out=stat[0:np_, t:t + 1]),
             reads=[dsrc], writes=[dstat, K.djunk] if t == 0 else [K.djunk], adds=[dstat] if t else ())
    P.op("dve", lambda e: e.tensor_scalar(out=stat[0:np_, n:2 * n], in0=stat[0:np_, 0:n], scalar1=1.0 / width, scalar2=1e-6,
                                          op0=ALU.mult, op1=ALU.add), reads=[dstat], writes=[dstat])
    P.op("act", lambda e: e.sqrt(stat[0:np_, n:2 * n], stat[0:np_, n:2 * n]), reads=[dstat], writes=[dstat])
    P.op("dve", lambda e: e.reciprocal(stat[0:np_, n:2 * n], stat[0:np_, n:2 * n]), reads=[dstat], writes=[dstat])
    for t, s in enumerate(srcs):
        P.op("dve", lambda e, s=s, t=t: e.scalar_tensor_tensor(out=outb[0:np_, t, :], in0=s, scalar=stat[0:np_, n + t:n + t + 1],
                                                               in1=g_ap[0:np_, :], op0=ALU.mult, op1=ALU.mult),
             reads=[dsrc, dstat, dg], writes=[doutb] if t == 0 else (), adds=[doutb] if t else ())


def transpose_block(K, inb, dinb, ntile, nchunk, out3, dout, np_=128, col0=0):
    P = K.P
    per = 1024 // (ntile * np_)
    first = True
    for c0 in range(0, nchunk, per):
        pT, dpT = K.pT.next()
        cs = range(c0, min(nchunk, c0 + per))
        for j, c in enumerate(cs):
            for t in range(ntile):
                o = (j * ntile + t) * np_
                P.op("pe", lambda e, o=o, c=c, t=t: e.transpose(pT[:, o:o + np_], inb[0:np_, t, c * 128:(c + 1) * 128],
                                                                K.ident[0:np_, 0:np_]),
                     reads=[dinb, K.dconst], writes=[dpT] if (j == 0 and t == 0) else (),
                     adds=() if (j == 0 and t == 0) else [dpT])
        w = ntile * np_
        nn = len(cs)
        copy_op(K, _alt(K), out3[:, c0:c0 + nn, col0:col0 + w],
                pT[:, 0:nn * w].rearrange("p (c w) -> p c w", w=w), reads=[dpT],
                writes=[dout] if first else (), adds=() if first else [dout])
        first = False


def emit_y(K, srcs, dsrc, g_ap, dg, row0, tok0):
    P = K.P
    yb, dyb = K.yb.next()
    norm_tiles(K, srcs, dsrc, yb, dyb, g_ap, dg, 512)
    ys, dys = K.ystage.next()
    transpose_block(K, yb, dyb, 4, 4, ys, dys)
    P.dma("pool", lambda e: e.dma_start(out=K.yT_d[row0:row0 + 512, tok0:tok0 + 512].rearrange("(c p) t -> p c t", p=128),
                                        in_=ys[:, :, :]), reads=[dys], adds=[K.dyT])


def phase_A(K):
    nc, P = K.nc, K.P
    with ExitStack() as st:
        T = lambda name, shape, dt: st.enter_context(nc.sbuf_tensor("sb_" + name, shape, dt))
        w = T("w_in", [128, 8, 2560], BF16); dw = Dep()
        for kc in range(8):
            for h2 in range(2):
                P.dma("pool", lambda e, kc=kc, h2=h2: e.dma_start(out=w[:, kc, h2 * 1280:(h2 + 1) * 1280],
                                                                  in_=K.w_in[kc * 128:(kc + 1) * 128, h2 * 1280:(h2 + 1) * 1280]),
                      adds=[dw])
        gm = T("gmix", [128, 1024], F32); dgm = Dep()
        P.dma("sp", lambda e: e.dma_start(out=gm[:, :], in_=K.gmix[:, :]), writes=[dgm])
        xr = Ring([T("xr%d" % i, [128, 4, 1024], F32) for i in range(2)])
        hb = Ring([T("hb%d" % i, [128, 4, 1024], BF16) for i in range(2)])
        hT = Ring([T("hT%d" % i, [128, 8, 512], BF16) for i in range(2)])
        stg = Ring([T("stg%d" % i, [128, 512], BF16) for i in range(4)])
        vst = Ring([T("vst%d" % i, [128, 8, 65], BF16) for i in range(2)])
        for v, dv in zip(vst.items, vst.deps):
            P.op("pool", lambda e, v=v: e.memset(v[:, :, :], 1.0), writes=[dv])
        blocks = [("core", i) for i in range(16)] + [("kv", i) for i in range(KVTOK // 512)]

        def load(bi):
            kind, i = blocks[bi]
            src = K.x_core if kind == "core" else K.x_kv
            x_, dx_ = xr.next()
            P.dma("sp", lambda e: e.dma_start(out=x_[:, :, :], in_=src[i * 512:(i + 1) * 512, :].rearrange("(t p) d -> p t d", p=128)),
                  writes=[dx_])
            return x_, dx_
        def norm_steps(xd):
            x_, dx_ = xd
            h_, dh_ = hb.next()
            stat, dstat = K.stat.next()
            steps = []
            for t in range(4):
                steps.append(lambda t=t: P.op("act", lambda e: e.activation(out=K.junk[:, 0:1024], in_=x_[:, t, :], func=AF.Square, accum_out=stat[:, t:t + 1]),
                                              reads=[dx_], writes=[dstat, K.djunk] if t == 0 else [K.djunk], adds=[dstat] if t else ()))

            def rstd():
                P.op("dve", lambda e: e.tensor_scalar(out=stat[:, 4:8], in0=stat[:, 0:4], scalar1=1.0 / 1024, scalar2=1e-6, op0=ALU.mult, op1=ALU.add),
                     reads=[dstat], writes=[dstat])
                P.op("act", lambda e: e.sqrt(stat[:, 4:8], stat[:, 4:8]), reads=[dstat], writes=[dstat])
                P.op("dve", lambda e: e.reciprocal(stat[:, 4:8], stat[:, 4:8]), reads=[dstat], writes=[dstat])
            steps.append(rstd)
            for t in range(4):
                steps.append(lambda t=t: P.op("dve", lambda e: e.scalar_tensor_tensor(out=h_[:, t, :], in0=x_[:, t, :], scalar=stat[:, 4 + t:5 + t], in1=gm[:, :],
                                                                                     op0=ALU.mult, op1=ALU.mult),
                                              reads=[dx_, dstat, dgm], writes=[dh_] if t == 0 else (), adds=[dh_] if t else ()))
            return (h_, dh_), steps
        X = {0: load(0)}
        if len(blocks) > 1:
            X[1] = load(1)
        H = {}
        H[0], st0 = norm_steps(X[0])
        for f_ in st0:
            f_()
        for bi, (kind, i) in enumerate(blocks):
            h_, dh_ = H[bi]
            hT_, dhT_ = hT.next()
            transpose_block(K, h_, dh_, 4, 8, hT_, dhT_)
            pend = []
            if bi + 1 < len(blocks):
                H[bi + 1], pend = norm_steps(X[bi + 1])
            if bi + 2 < len(blocks):
                X[bi + 2] = load(bi + 2)
            if kind == "core":
                fcs = [12, 13, 14, 15]
                if i < 8:
                    fcs = [0, 1, 2, 3, 16, 17, 18, 19] + fcs
            else:
                fcs = [4, 5, 6, 7]
            for fc in fcs:
                pf, dpf = K.pF.next()
                for kc in range(8):
                    P.op("pe", lambda e, kc=kc, fc=fc, pf=pf: e.matmul(pf[:, :], lhsT=w[:, kc, fc * 128:(fc + 1) * 128], rhs=hT_[:, kc, :],
                                                                       start=(kc == 0), stop=(kc == 7)),
                         reads=[dw, dhT_], writes=[dpf] if kc == 0 else (), adds=() if kc == 0 else [dpf])
                s_, ds_ = stg.next()
                copy_op(K, _alt(K), s_[:, :], pf[:, :], reads=[dpf], writes=[ds_])
                if fc < 4:
                    dst, dd = K.qT_d[fc * 128:(fc + 1) * 128, i * 512:(i + 1) * 512], K.dqT
                elif fc < 8:
                    dst, dd = K.kT_d[(fc - 4) * 128:(fc - 3) * 128, i * 512:(i + 1) * 512], K.dkT
                elif fc < 16:
                    dst, dd = K.uT_d[(fc - 12) * 128:(fc - 11) * 128, i * 512:(i + 1) * 512], K.duT
                else:
                    dst, dd = K.qmT_d[(fc - 16) * 128:(fc - 15) * 128, i * 512:(i + 1) * 512], K.dqmT
                P.dma("pool", lambda e, dst=dst, s_=s_: e.dma_start(out=dst, in_=s_[:, :]), reads=[ds_], adds=[dd])
                for _ in range(-(-9 // max(1, len(fcs)))):
                    if pend:
                        pend.pop(0)()
            if kind == "kv":
                for t in range(4):
                    pf, dpf = K.pF.next()
                    for kc in range(8):
                        P.op("pe", lambda e, kc=kc, t=t, pf=pf: e.matmul(pf[:, :], lhsT=hT_[:, kc, t * 128:(t + 1) * 128], rhs=w[:, kc, 1024:1536],
                                                                         start=(kc == 0), stop=(kc == 7)),
                             reads=[dw, dhT_], writes=[dpf] if kc == 0 else (), adds=() if kc == 0 else [dpf])
                    v_, dv_ = vst.next()
                    copy_op(K, _alt(K), v_[:, :, 0:64], pf[:, :].rearrange("p (h d) -> p h d", d=64), reads=[dpf], writes=[dv_])
                    r0 = i * 512 + t * 128
                    P.dma("pool", lambda e, r0=r0, v_=v_: e.dma_start(out=K.V_d[r0:r0 + 128, :], in_=v_[:, :, :].rearrange("p h d -> p (h d)")),
                          reads=[dv_], adds=[K.dV])
            while pend:
                pend.pop(0)()


def phase_MEM(K):
    nc, P = K.nc, K.P
    with ExitStack() as st:
        T = lambda name, shape, dt: st.enter_context(nc.sbuf_tensor("sb_" + name, shape, dt))
        K.yb = Ring([T("ybm%d" % i, [128, 4, 512], BF16) for i in range(2)])
        K.ystage = Ring([T("ystm%d" % i, [128, 4, 512], BF16) for i in range(2)])
        wkv = T("wkv", [128, 8, 1024], BF16); dwkv = Dep()
        for kc in range(8):
            P.dma("pool", lambda e, kc=kc: e.dma_start(out=wkv[:, kc, :], in_=K.w_kv[kc * 128:(kc + 1) * 128, :]), adds=[dwkv])
        gme = T("gmem", [128, 1024], F32); dgme = Dep()
        P.dma("sp", lambda e: e.dma_start(out=gme[:, :], in_=K.gmem[:, :]), writes=[dgme])
        gg = T("ggm", [128, 512], F32); dgg = Dep()
        P.dma("sp", lambda e: e.dma_start(out=gg[:, :], in_=K.ggrp[:, 1024:1536]), writes=[dgg])
        mx = T("memx", [128, 2, 1024], F32); dmx = Dep()
        P.dma("sp", lambda e: e.dma_start(out=mx[:, :, :], in_=K.mem[:, :].rearrange("(t p) d -> p t d", p=128)), writes=[dmx])
        mb = T("memb", [128, 2, 1024], BF16); dmb = Dep()
        norm_tiles(K, [mx[:, t, :] for t in range(2)], dmx, mb, dmb, gme, dgme, 1024)
        memT = T("memT", [128, 8, 256], BF16); dmemT = Dep()
        transpose_block(K, mb, dmb, 2, 8, memT, dmemT)
        if CUT == 1: return
        kmT = T("kmT", [128, 4, 256], BF16); dkmT = Dep()
        for h in range(4):
            pf, dpf = K.pF.next()
            for kc in range(8):
                P.op("pe", lambda e, kc=kc, h=h, pf=pf: e.matmul(pf[:, 0:256], lhsT=wkv[:, kc, h * 128:(h + 1) * 128], rhs=memT[:, kc, :],
                                                                 start=(kc == 0), stop=(kc == 7)),
                     reads=[dwkv, dmemT], writes=[dpf] if kc == 0 else (), adds=() if kc == 0 else [dpf])
            copy_op(K, _alt(K), kmT[:, h, :], pf[:, 0:256], reads=[dpf], writes=[dkmT] if h == 0 else (), adds=[dkmT] if h else ())
        if CUT == 2: return
        Vm = T("Vm", [128, 2, 4, 129], BF16); dVm = Dep()
        P.op("pool", lambda e: e.memset(Vm[:, :, :, :], 1.0), writes=[dVm])
        for mt in range(2):
            pf, dpf = K.pF.next()
            for kc in range(8):
                P.op("pe", lambda e, kc=kc, mt=mt, pf=pf: e.matmul(pf[:, :], lhsT=memT[:, kc, mt * 128:(mt + 1) * 128], rhs=wkv[:, kc, 512:1024],
                                                                   start=(kc == 0), stop=(kc == 7)),
                     reads=[dwkv, dmemT], writes=[dpf] if kc == 0 else (), adds=() if kc == 0 else [dpf])
            copy_op(K, _alt(K), Vm[:, mt, :, 0:128], pf[:, :].rearrange("p (h d) -> p h d", d=128), reads=[dpf, dVm], writes=[dVm])
        if CUT == 3: return
        qm = Ring([T("qm%d" % i, [128, 4, 512], BF16) for i in range(2)])
        PT = Ring([T("PTm%d" % i, [128, 2, 512], BF16) for i in range(2)])
        ym = Ring([T("ym%d" % i, [128, 4, 512], F32) for i in range(2)])
        rd = Ring([T("rdm%d" % i, [128, 4], F32) for i in range(4)])
        def load_qm(blk):
            q_, dq_ = qm.next()
            P.dma("sp", lambda e: e.dma_start(out=q_[:, :, :], in_=K.qmT_d[:, blk * 512:(blk + 1) * 512].rearrange("(h p) t -> p h t", p=128)),
                  reads=[K.dqmT], writes=[dq_])
            return q_, dq_
        nq = load_qm(0)
        for blk in range(8):
            q_, dq_ = nq
            if blk < 7:
                nq = load_qm(blk + 1)
            y_, dy_ = ym.next()

            def s1(h):
                pt_, dpt_ = PT.next()
                for mt in range(2):
                    pf, dpf = K.pF.next()
                    P.op("pe", lambda e, mt=mt, pf=pf: e.matmul(pf[:, :], lhsT=kmT[:, h, mt * 128:(mt + 1) * 128], rhs=q_[:, h, :], start=True, stop=True),
                         reads=[dkmT, dq_], writes=[dpf])
                    P.op("act", lambda e, mt=mt, pf=pf: e.activation(out=pt_[:, mt, :], in_=pf[:, :], func=AF.Exp, scale=float(128 ** -0.5)),
                         reads=[dpf], writes=[dpt_] if mt == 0 else (), adds=[dpt_] if mt else ())
                return pt_, dpt_

            def s2(h, pt_, dpt_):
                pa, dpa = K.pF.next()
                pb, dpb = K.pF.next()
                for t in range(4):
                    po = pa[:, t * 129:(t + 1) * 129] if t < 3 else pb[:, 0:129]
                    dpo = dpa if t < 3 else dpb
                    for mt in range(2):
                        P.op("pe", lambda e, mt=mt, t=t, po=po: e.matmul(po, lhsT=pt_[:, mt, t * 128:(t + 1) * 128], rhs=Vm[:, mt, h, :],
                                                                         start=(mt == 0), stop=(mt == 1)),
                             reads=[dpt_, dVm], writes=[dpo] if (mt == 0 and t in (0, 3)) else (),
                             adds=() if (mt == 0 and t in (0, 3)) else [dpo])
                r_, dr_ = rd.next()
                P.op("dve", lambda e: e.reciprocal(r_[:, 0:3], pa[:, 0:387].rearrange("p (t c) -> p t c", c=129)[:, :, 128]),
                     reads=[dpa], writes=[dr_])
                P.op("dve", lambda e: e.reciprocal(r_[:, 3:4], pb[:, 128:129]), reads=[dpb], adds=[dr_])
                for t in range(4):
                    po = pa[:, t * 129:t * 129 + 128] if t < 3 else pb[:, 0:128]
                    dpo = dpa if t < 3 else dpb
                    if t < 3:
                        P.op("act", lambda e, t=t, po=po: e.mul(y_[:, t, h * 128:(h + 1) * 128], po, r_[:, t:t + 1]),
                             reads=[dpo, dr_], writes=[dy_] if (h == 0 and t == 0) else (), adds=() if (h == 0 and t == 0) else [dy_])
                    else:
                        P.op("dve", lambda e, t=t, po=po: e.tensor_scalar_mul(y_[:, t, h * 128:(h + 1) * 128], po, r_[:, t:t + 1]),
                             reads=[dpo, dr_], adds=[dy_])
            cur = s1(0)
            for h in range(4):
                nxt_ = s1(h + 1) if h < 3 else None
                s2(h, *cur)
                cur = nxt_
            if CUT == 6: return
            emit_y(K, [y_[:, t, :] for t in range(4)], dy_, gg, dgg, 1024, blk * 512)
            if CUT == 7: return


def phase_NA(K):
    nc, P = K.nc, K.P
    with ExitStack() as st:
        T = lambda name, shape, dt: st.enter_context(nc.sbuf_tensor("sb_" + name, shape, dt))
        qTr = Ring([T("qT%d" % i, [128, 4, 512], BF16) for i in range(2)])
        kT = T("kT", [128, 4, KVTOK], BF16); dk = Dep()
        for fc in range(4):
            P.dma("sp", lambda e, fc=fc: e.dma_start(out=kT[:, fc, :], in_=K.kT_d[fc * 128:(fc + 1) * 128, :]), reads=[K.dkT], adds=[dk])
        pO = Ring([(K.pF.items[0], K.pF.items[1])])
        pO.deps = [(K.pF.deps[0], K.pF.deps[1])]
        pS = Ring([K.pF.items[i] for i in (2, 3, 4, 5)]); pS.deps = [K.pF.deps[i] for i in (2, 3, 4, 5)]
        maskf = T("maskf", [128, 2048], BF16); dmask = Dep()
        P.dma("sp", lambda e: e.dma_start(out=maskf[:, :], in_=K.na_mask[:, :]), writes=[dmask])
        gg = T("ggn", [64, 512], F32); dgg = Dep()
        P.dma("sp", lambda e: e.dma_start(out=gg[:, :], in_=K.ggrp[0:64, 0:512]), writes=[dgg])
        Vp = [T("Vp%d" % i, [128, 520], BF16) for i in range(16)]; dVp = [Dep() for _ in range(16)]
        bias = Ring([T("nab%d" % i, [128, 2048], F32) for i in range(2)])
        EB = Ring([T("EB%d" % i, [128, 2048], BF16) for i in range(2)])
        eS = Ring([T("eS%d" % i, [128, 512], F32) for i in range(4)])
        PT = Ring([T("PTn%d" % i, [128, 512], BF16) for i in range(5)])
        yn = Ring([T("yn%d" % i, [64, 512], F32) for i in range(2)])
        ynb = Ring([T("ynb%d" % i, [64, 1, 512], BF16) for i in range(2)])
        rd = Ring([T("rdn%d" % i, [64, 8], F32) for i in range(2)])
        ys = Ring([T("ysn%d" % i, [128, 4, 512], BF16) for i in range(2)])
        ploaded = 0

        def load_q(l0):
            qT, dq = qTr.next()
            P.dma("sp", lambda e: e.dma_start(out=qT[:, :, :], in_=K.qT_d[:, l0 * 64:l0 * 64 + 512].rearrange("(c p) t -> p c t", p=128)),
                  reads=[K.dqT], writes=[dq])
            return qT, dq

        def load_bias(ti_):
            b_, db_ = bias.next()
            P.dma("sp", lambda e: e.dma_start(out=b_[:, :], in_=K.na_bias[ti_, :, :]), writes=[db_])
            return b_, db_
        own_tab = lambda lq: lq <= 4 or lq >= 60
        ntab = sum(1 for lq in range(64) if own_tab(lq))
        nb = load_bias(0); ti_ = 0
        pending = None; curT = [None]
        for lq in range(64):
            while ploaded < lq + 7:
                j = ploaded
                P.dma("sp", lambda e, j=j: e.dma_start(out=Vp[j % 16][:, :], in_=K.V_d[j * 64:j * 64 + 128, :]), reads=[K.dV], writes=[dVp[j % 16]])
                ploaded += 1
            if lq % 8 == 0:
                if lq == 0:
                    nq_ = load_q(0)
                qT, dq = nq_
            if lq % 8 == 4 and lq + 4 < 64:
                nq_ = load_q(lq + 4)
            if own_tab(lq):
                b_, db_ = nb
                P.op("act", lambda e, b_=b_: e.activation(out=b_[:, :], in_=b_[:, :], func=AF.Exp), reads=[db_], writes=[db_])
                eb_, deb_ = EB.next()
                P.op("dve", lambda e, b_=b_, eb_=eb_: e.tensor_tensor(out=eb_[:, :], in0=b_[:, :], in1=maskf[:, :], op=ALU.mult), reads=[db_, dmask], writes=[deb_])
                ti_ += 1
                if ti_ < ntab:
                    nb = load_bias(ti_)
            (pa, pb), (dpa, dpb) = pO.next()

            def qk(hp):
                ps, dps = pS.next()
                first = True
                for hh in range(2):
                    h = HORD[2 * hp + hh]
                    pb0 = 64 * (h % 2); fc = h // 2
                    for m in range(4):
                        k0 = (lq + 2 * m) * 64
                        P.op("pe", lambda e, m=m, k0=k0, hh=hh, pb0=pb0, fc=fc, ps=ps: e.matmul(
                            ps[:, hh * 256 + m * 64:hh * 256 + (m + 1) * 64], lhsT=kT[pb0:pb0 + 64, fc, k0:k0 + 128],
                            rhs=qT[pb0:pb0 + 64, fc, (lq % 8) * 64:(lq % 8 + 1) * 64], start=True, stop=True),
                            reads=[dk, dq], writes=[dps] if first else (), adds=() if first else [dps])
                        first = False
                e_, de_ = eS.next()
                P.op("act", lambda e, ps=ps, e_=e_: e.activation(out=e_[:, :], in_=ps[:, :], func=AF.Exp, scale=0.125), reads=[dps], writes=[de_])
                p_, dp_ = PT.next()
                P.op("dve", lambda e, e_=e_, p_=p_: e.tensor_tensor(out=p_[:, :], in0=e_[:, :], in1=eb_[:, hp * 512:(hp + 1) * 512], op=ALU.mult),
                     reads=[de_, deb_], writes=[dp_])
                return p_, dp_

            def pv(hp, p_, dp_):
                for hh in range(2):
                    h = HORD[2 * hp + hh]
                    po_t, dpo = (pa, dpa) if h < 4 else (pb, dpb)
                    c0 = (h % 4) * 65
                    for m in range(4):
                        kr = lq + 2 * m
                        P.op("pe", lambda e, m=m, kr=kr, h=h, hh=hh, po_t=po_t, c0=c0: e.matmul(
                            po_t[0:64, c0:c0 + 65], lhsT=p_[:, hh * 256 + m * 64:hh * 256 + (m + 1) * 64],
                            rhs=Vp[kr % 16][:, h * 65:(h + 1) * 65], start=(m == 0), stop=(m == 3)),
                            reads=[dp_, dVp[kr % 16]], writes=[dpo] if (m == 0 and h % 4 == 0) else (),
                            adds=() if (m == 0 and h % 4 == 0) else [dpo])
            curs = [qk(hp) for hp in range(4)]
            if pending is not None:
                pending(); pending = None
            for hp in range(4):
                pv(hp, *curs[hp])
            r_, dr_ = rd.next()
            P.op("dve", lambda e, r_=r_, pa=pa: e.reciprocal(r_[:, 0:4], pa[0:64, 0:260].rearrange("p (h c) -> p h c", c=65)[:, :, 64]), reads=[dpa], writes=[dr_])
            P.op("dve", lambda e, r_=r_, pb=pb: e.reciprocal(r_[:, 4:8], pb[0:64, 0:260].rearrange("p (h c) -> p h c", c=65)[:, :, 64]), reads=[dpb], adds=[dr_])
            y_, dy_ = yn.next()
            for h in range(8):
                po_t, dpo = (pa, dpa) if h < 4 else (pb, dpb)
                c0 = (h % 4) * 65
                P.op("dve", lambda e, h=h, po_t=po_t, c0=c0, y_=y_, r_=r_: e.tensor_scalar_mul(y_[:, h * 64:(h + 1) * 64], po_t[0:64, c0:c0 + 64], r_[:, h:h + 1]),
                     reads=[dpo, dr_], writes=[dy_] if h == 0 else (), adds=[dy_] if h else ())
            yb_, dyb_ = ynb.next()
            norm_tiles(K, [y_[:, :]], dy_, yb_, dyb_, gg, dgg, 512, np_=64)

            def row_end(lq=lq, yb_=yb_, dyb_=dyb_):
                sub = lq % 8
                if sub == 0:
                    pTa, dpTa = K.pT.next()
                    pTb, dpTb = K.pT.next()
                    curT[0] = (pTa, dpTa, pTb, dpTb)
                pTa, dpTa, pTb, dpTb = curT[0]
                for fc in range(4):
                    pt, dpt = (pTa, dpTa) if fc < 2 else (pTb, dpTb)
                    o = (fc % 2) * 512 + sub * 64
                    P.op("pe", lambda e, fc=fc, pt=pt, o=o: e.transpose(pt[:, o:o + 64], yb_[:, 0, fc * 128:(fc + 1) * 128], K.ident[0:64, 0:64]),
                         reads=[dyb_, K.dconst], writes=[dpt] if (sub == 0 and fc % 2 == 0) else (),
                         adds=() if (sub == 0 and fc % 2 == 0) else [dpt])
                if sub == 7:
                    s_, ds_ = ys.next()
                    copy_op(K, "act", s_[:, 0:2, :], pTa[:, :].rearrange("p (c w) -> p c w", w=512), reads=[dpTa], writes=[ds_])
                    copy_op(K, "dve", s_[:, 2:4, :], pTb[:, :].rearrange("p (c w) -> p c w", w=512), reads=[dpTb], adds=[ds_])
                    t0 = (lq // 8) * 512
                    P.dma("pool", lambda e, t0=t0, s_=s_: e.dma_start(out=K.yT_d[0:512, t0:t0 + 512].rearrange("(c p) t -> p c t", p=128), in_=s_[:, :, :]),
                          reads=[ds_], adds=[K.dyT])
            pending = row_end
        pending()


def phase_FT(K):
    nc, P = K.nc, K.P
    with ExitStack() as st:
        T = lambda name, shape, dt: st.enter_context(nc.sbuf_tensor("sb_" + name, shape, dt))
        uT = T("uT", [128, 4, SEQ], BF16); du = Dep()
        for g in range(4):
            for hh in range(2):
                P.dma("sp", lambda e, g=g, hh=hh: e.dma_start(out=uT[:, g, hh * 4096:(hh + 1) * 4096], in_=K.uT_d[g * 128:(g + 1) * 128, hh * 4096:(hh + 1) * 4096]),
                      reads=[K.duT], adds=[du])
        cs4 = T("cs4", [128, 512], BF16); E = T("Etw", [128, 64, 2, 128], BF16); dc = Dep()
        P.dma("sp", lambda e: e.dma_start(out=cs4[:, :], in_=K.cs4[:, :]), adds=[dc])
        P.dma("sp", lambda e: e.dma_start(out=E[:, :, :, :], in_=K.etw[:, :, :, :]), adds=[dc])
        Z = Ring([T("Z%d" % i, [128, 512], BF16) for i in range(3)])
        Bs = Ring([T("Bs%d" % i, [128, 2, 128], BF16) for i in range(3)])
        def st1(s1, g):
            pf, dpf = K.pF.next()
            lhs = uT[:, g, :].rearrange("p (s2 s1) -> p s1 s2", s1=64)[:, s1, :]
            P.op("pe", lambda e, lhs=lhs, pf=pf: e.matmul(pf[:, :], lhsT=lhs, rhs=cs4[:, :], start=True, stop=True), reads=[du, dc], writes=[dpf])
            z_, dz_ = Z.next()
            copy_op(K, _alt(K), z_[:, :], pf[:, :], reads=[dpf], writes=[dz_])
            return z_, dz_

        def st2(s1, g, z_, dz_):
            pf2, dpf2 = K.pF.next()
            P.op("pe", lambda e, pf2=pf2: e.matmul(pf2[:, 0:256], lhsT=E[:, s1, 0, :], rhs=z_[:, 0:256], start=True, stop=False), reads=[dz_, dc], writes=[dpf2])
            P.op("pe", lambda e, pf2=pf2: e.matmul(pf2[:, 0:256], lhsT=E[:, s1, 1, :], rhs=z_[:, 256:512], start=False, stop=True), reads=[dz_, dc], adds=[dpf2])
            b_, db_ = Bs.next()
            copy_op(K, _alt(K), b_[:, :, :], pf2[:, 0:256].rearrange("p (r c) -> p r c", c=128), reads=[dpf2], writes=[db_])
            P.dma("pool", lambda e, g=g, b_=b_: e.dma_start(out=K.B_d[g, :, s1, :, :].rearrange("r k c -> k r c"), in_=b_[:, :, :]),
                  reads=[db_], adds=[K.dB])
        units = [(s1, g) for s1 in range(64) for g in range(4)]
        zc = st1(*units[0])
        for i, u in enumerate(units):
            zn = st1(*units[i + 1]) if i + 1 < len(units) else None
            st2(*u, *zc)
            zc = zn
        P.barrier()
    with ExitStack() as st:
        T = lambda name, shape, dt: st.enter_context(nc.sbuf_tensor("sb_" + name, shape, dt))
        K.yb = Ring([T("ybf%d" % i, [128, 4, 512], BF16) for i in range(2)])
        K.ystage = Ring([T("ystf%d" % i, [128, 4, 512], BF16) for i in range(2)])
        Dm = T("Dm", [128, 32], BF16); dDm = Dep()
        P.dma("sp", lambda e: e.dma_start(out=Dm[:, :], in_=K.dmat[:, :]), writes=[dDm])
        gg = T("ggf", [128, 512], F32); dgg = Dep()
        P.dma("sp", lambda e: e.dma_start(out=gg[:, :], in_=K.ggrp[:, 512:1024]), writes=[dgg])
        Bt = Ring([T("Bt%d" % i, [128, 128, 128], BF16) for i in range(2)])
        yft = T("yft", [128, 32, 512], F32); dyft = Dep()
        def load_bt(g):
            bt_, dbt_ = Bt.next()
            for hh in range(2):
                P.dma("sp", lambda e, hh=hh: e.dma_start(out=bt_[hh * 64:(hh + 1) * 64, :, :], in_=K.B_d[g, hh, :, :, :]), reads=[K.dB],
                      writes=[dbt_] if hh == 0 else (), adds=[dbt_] if hh else ())
            return bt_, dbt_
        nbt = load_bt(0)
        for g in range(4):
            bt_, dbt_ = nbt
            if g < 3:
                nbt = load_bt(g + 1)
            for cb in range(8):
                pf, dpf = K.pF.next()
                for cc in range(16):
                    c = cb * 16 + cc
                    P.op("pe", lambda e, c=c, cc=cc, pf=pf: e.matmul(pf[:, cc * 32:(cc + 1) * 32], lhsT=bt_[:, :, c], rhs=Dm[:, :], start=True, stop=True),
                         reads=[dbt_, dDm], writes=[dpf] if cc == 0 else (), adds=() if cc == 0 else [dpf])
                c0 = g * 128 + cb * 16
                copy_op(K, _alt(K), yft[:, :, c0:c0 + 16], pf[:, :].rearrange("p (c k) -> p k c", k=32), reads=[dpf],
                        writes=[dyft] if (g == 0 and cb == 0) else (), adds=() if (g == 0 and cb == 0) else [dyft])
        for k4 in range(8):
            emit_y(K, [yft[:, k4 * 4 + t, :] for t in range(4)], dyft, gg, dgg, 512, k4 * 512)


def phase_OUT(K):
    nc, P = K.nc, K.P
    with ExitStack() as st:
        T = lambda name, shape, dt: st.enter_context(nc.sbuf_tensor("sb_" + name, shape, dt))
        wo = T("wo", [128, 12, 1024], BF16); dwo = Dep()
        for fc in range(12):
            P.dma("pool", lambda e, fc=fc: e.dma_start(out=wo[:, fc, :], in_=K.w_out[fc * 128:(fc + 1) * 128, :]), adds=[dwo])
        rw = T("rw", [128, 8, 32], BF16); drw = Dep()
        P.dma("pool", lambda e: e.dma_start(out=rw[:, :, :], in_=K.router_w[:, :].rearrange("(c p) e -> p c e", p=128)), writes=[drw])
        cst = T("ocst", [128, 1024 + 32 + 32], F32); dcst = Dep()
        gf = cst[:, 0:1024]; rb = cst[:, 1024:1056]; iota = cst[:, 1056:1088]
        P.dma("sp", lambda e: e.dma_start(out=gf, in_=K.gffn[:, :]), adds=[dcst])
        P.dma("sp", lambda e: e.dma_start(out=rb, in_=K.router_b[:, :]), adds=[dcst])
        P.dma("sp", lambda e: e.dma_start(out=iota, in_=K.iota[:, :]), adds=[dcst])
        LO = T("LO", [128, 2, 128], BF16); dLO = Dep()
        P.dma("sp", lambda e: e.dma_start(out=LO[:, :, :], in_=K.lo[:, :, :]), writes=[dLO])
        base = T("base", [128, 32], F32); dbase = Dep()
        P.op("pool", lambda e: e.memset(base[:, :], 0.0), writes=[dbase])
        yT = Ring([T("yTo%d" % i, [128, 12, 512], BF16) for i in range(2)])
        xr = Ring([T("xro%d" % i, [128, 4, 1024], F32) for i in range(2)])
        x1 = Ring([T("x1o%d" % i, [128, 1024], F32) for i in range(2)])
        h2all = T("h2all", [128, 32, 1024], BF16); dh2 = [Dep() for _ in range(32)]
        ekall = T("ekall", [128, 128], F32); slall = T("slall", [128, 128], F32); dek = Dep()
        rofs = T("rofs", [128, 8], F32); drofs = Dep()
        P.dma("sp", lambda e: e.dma_start(out=rofs[:, :], in_=K.rowoff[:, :]), writes=[drofs])
        h2T = Ring([T("h2T%d" % i, [128, 8, 128], BF16) for i in range(2)])
        sm = Ring([T("sm%d" % i, [128, 160], F32) for i in range(2)])
        Mb = Ring([T("Mb%d" % i, [128, 32], BF16) for i in range(2)])

        def load(blk):
            y_, dy_ = yT.next()
            P.dma("sp", lambda e: e.dma_start(out=y_[:, :, :], in_=K.yT_d[:, blk * 512:(blk + 1) * 512].rearrange("(c p) t -> p c t", p=128)),
                  reads=[K.dyT], writes=[dy_])
            x_, dx_ = xr.next()
            P.dma("sp", lambda e: e.dma_start(out=x_[:, :, :], in_=K.x_core[blk * 512:(blk + 1) * 512, :].rearrange("(t p) d -> p t d", p=128)),
                  writes=[dx_])
            return y_, dy_, x_, dx_
        tiles = [(blk, t) for blk in range(8) for t in range(4)]
        loaded = {}

        def get_blk(blk):
            if blk not in loaded:
                loaded[blk] = load(blk)
            return loaded[blk]
        get_blk(0)

        def stageA(blk, t):
            ti = blk * 4 + t
            y_, dy_, x_, dx_ = get_blk(blk)
            if t == 0 and blk < 7:
                get_blk(blk + 1)
            x1_, dx1_ = x1.next()
            for dh in range(2):
                pf, dpf = K.pF.next()
                for fc in range(12):
                    P.op("pe", lambda e, fc=fc, dh=dh, pf=pf: e.matmul(pf[:, :], lhsT=y_[:, fc, t * 128:(t + 1) * 128], rhs=wo[:, fc, dh * 512:(dh + 1) * 512],
                                                                       start=(fc == 0), stop=(fc == 11)),
                         reads=[dy_, dwo], writes=[dpf] if fc == 0 else (), adds=() if fc == 0 else [dpf])
                P.op("dve", lambda e, dh=dh, pf=pf: e.tensor_tensor(out=x1_[:, dh * 512:(dh + 1) * 512], in0=pf[:, :], in1=x_[:, t, dh * 512:(dh + 1) * 512], op=ALU.add),
                     reads=[dpf, dx_], writes=[dx1_] if dh == 0 else (), adds=[dx1_] if dh else ())
            P.dma("pool", lambda e: e.dma_start(out=K.x1_d[ti * 128:(ti + 1) * 128, :], in_=x1_[:, :]), reads=[dx1_], adds=[K.dx1])
            stat, dstat = K.stat.next()
            P.op("act", lambda e: e.activation(out=K.junk[:, 0:1024], in_=x1_[:, :], func=AF.Square, accum_out=stat[:, 0:1]),
                 reads=[dx1_], writes=[dstat, K.djunk])
            return ti, x1_, dx1_, stat, dstat

        def stageB(ti, x1_, dx1_, stat, dstat):
            dhb_ = dh2[ti]
            hb3 = h2all[:, ti:ti + 1, :]
            P.op("dve", lambda e: e.tensor_scalar(out=stat[:, 1:2], in0=stat[:, 0:1], scalar1=1.0 / 1024, scalar2=1e-6, op0=ALU.mult, op1=ALU.add),
                 reads=[dstat], writes=[dstat])
            P.op("act", lambda e: e.sqrt(stat[:, 1:2], stat[:, 1:2]), reads=[dstat], writes=[dstat])
            P.op("dve", lambda e: e.reciprocal(stat[:, 1:2], stat[:, 1:2]), reads=[dstat], writes=[dstat])
            P.op("dve", lambda e: e.scalar_tensor_tensor(out=hb3[:, 0, :], in0=x1_[:, :], scalar=stat[:, 1:2], in1=gf, op0=ALU.mult, op1=ALU.mult),
                 reads=[dx1_, dstat, dcst], writes=[dhb_])
            hT_, dhT_ = h2T.next()
            transpose_block(K, hb3, dhb_, 1, 8, hT_, dhT_)
            pl, dpl = K.pF.next()
            for kc in range(8):
                P.op("pe", lambda e, kc=kc, pl=pl: e.matmul(pl[:, 0:32], lhsT=hT_[:, kc, :], rhs=rw[:, kc, :], start=(kc == 0), stop=(kc == 7)),
                     reads=[dhT_, drw], writes=[dpl] if kc == 0 else (), adds=() if kc == 0 else [dpl])
            s_, ds_ = sm.next()
            lg = s_[:, 0:32]
            P.op("dve", lambda e, pl=pl: e.tensor_tensor(out=lg, in0=pl[:, 0:32], in1=rb, op=ALU.add), reads=[dpl, dcst], writes=[ds_])

            def route(ti=ti, s_=s_, ds_=ds_):
                lg = s_[:, 0:32]; t8 = s_[:, 32:40]; slot = s_[:, 40:72]; junk = s_[:, 72:104]
                sl = s_[:, 104:108]; ek = s_[:, 108:112]
                nmx = s_[:, 120:121]; gs = s_[:, 121:122]; e4 = s_[:, 124:128]
                P.op("dve", lambda e: e.max(out=t8, in_=lg), reads=[ds_], writes=[ds_])
                m_, dm_ = Mb.next()
                P.op("dve", lambda e: e.tensor_scalar(out=m_[:, :], in0=lg, scalar1=s_[:, 35:36], scalar2=1.0, op0=ALU.is_ge, op1=ALU.mult), reads=[ds_], writes=[dm_])
                pp, dpp = K.pF.next()
                P.op("pe", lambda e, pp=pp: e.matmul(pp[:, 0:32], lhsT=LO[:, 0, :], rhs=m_[:, :], start=True, stop=True), reads=[dLO, dm_], writes=[dpp])
                P.op("pe", lambda e, pp=pp: e.matmul(pp[:, 32:64], lhsT=LO[:, 1, :], rhs=m_[:, :], start=True, stop=True), reads=[dLO, dm_], adds=[dpp])
                P.op("dve", lambda e, pp=pp: e.tensor_tensor(out=slot, in0=pp[:, 0:32], in1=base[:, :], op=ALU.add), reads=[dpp, dbase, ds_], writes=[ds_])
                P.op("dve", lambda e, pp=pp: e.tensor_tensor(out=base[:, :], in0=pp[:, 32:64], in1=base[:, :], op=ALU.add), reads=[dpp, dbase], writes=[dbase])
                for k in range(4):
                    P.op("dve", lambda e, k=k: e.scalar_tensor_tensor(out=junk, in0=lg, scalar=s_[:, 32 + k:33 + k], in1=slot, op0=ALU.is_equal, op1=ALU.mult,
                                                                      accum_out=sl[:, k:k + 1]), reads=[ds_], writes=[ds_])
                    P.op("dve", lambda e, k=k: e.scalar_tensor_tensor(out=junk, in0=lg, scalar=s_[:, 32 + k:33 + k], in1=iota, op0=ALU.is_equal, op1=ALU.mult,
                                                                      accum_out=ek[:, k:k + 1]), reads=[ds_, dcst], writes=[ds_])
                P.op("dve", lambda e, ti=ti: e.tensor_copy(ekall[:, ti * 4:ti * 4 + 4], ek), reads=[ds_], adds=[dek])
                P.op("dve", lambda e, ti=ti: e.tensor_copy(slall[:, ti * 4:ti * 4 + 4], sl), reads=[ds_], adds=[dek])
                P.op("dve", lambda e: e.tensor_scalar_mul(nmx, s_[:, 32:33], -1.0), reads=[ds_], writes=[ds_])
                P.op("act", lambda e: e.activation(out=e4, in_=s_[:, 32:36], func=AF.Exp, bias=nmx, scale=1.0, accum_out=gs), reads=[ds_], writes=[ds_])
                P.op("dve", lambda e: e.reciprocal(gs, gs), reads=[ds_], writes=[ds_])
                P.op("dve", lambda e, ti=ti: e.tensor_scalar_mul(K.gates_all[:, ti, :], e4, gs), reads=[ds_], adds=[K.dgates])
            return route
        pend_route = None
        curA = stageA(*tiles[0])
        for i in range(len(tiles)):
            nxtA = stageA(*tiles[i + 1]) if i + 1 < len(tiles) else None
            r = stageB(*curA)
            if pend_route is not None:
                pend_route()
            pend_route = r
            curA = nxtA
        pend_route()
        rt = T("rt", [128, 512], F32); drt = Dep()
        nblk = rt[:, 0:32]; c0 = rt[:, 32:64]; c1 = rt[:, 64:96]; pstart = rt[:, 96:128]; one32 = rt[:, 128:160]; j32 = rt[:, 160:192]
        bexp = rt[:, 192:256]; bex1024 = rt[:, 256:320]; bex128 = rt[:, 320:384]; tmp64 = rt[:, 384:448]
        P.op("dve", lambda e: e.memset(rt[:, :], 0.0), writes=[drt])
        P.op("dve", lambda e: e.memset(one32, 1.0), writes=[drt])
        for j in range(8):
            P.op("dve", lambda e, j=j: e.scalar_tensor_tensor(out=nblk, in0=base[:, :], scalar=float(BLK * j), in1=nblk, op0=ALU.is_gt, op1=ALU.add),
                 reads=[dbase, drt], writes=[drt])
        P.op("dve", lambda e: e.tensor_copy(c0, nblk), reads=[drt], writes=[drt])
        src, dst = c0, c1
        for sft in (1, 2, 4, 8, 16):
            P.op("dve", lambda e, src=src, dst=dst, sft=sft: e.tensor_copy(dst[:, 0:sft], src[:, 0:sft]), reads=[drt], writes=[drt])
            P.op("dve", lambda e, src=src, dst=dst, sft=sft: e.tensor_tensor(out=dst[:, sft:32], in0=src[:, sft:32], in1=src[:, 0:32 - sft], op=ALU.add),
                 reads=[drt], writes=[drt])
            src, dst = dst, src
        cb = src
        P.op("dve", lambda e: e.tensor_tensor(out=pstart, in0=cb, in1=nblk, op=ALU.subtract), reads=[drt], writes=[drt])
        P.op("dve", lambda e: e.tensor_scalar_mul(pstart, pstart, float(BLK)), reads=[drt], writes=[drt])
        dstf = T("dstf", [128, 128], F32); ddf = Dep()
        jbig = T("jbig", [128, 128 * 32], F32)
        for col in range(128):
            P.op("dve", lambda e, col=col: e.scalar_tensor_tensor(out=jbig[:, col * 32:(col + 1) * 32], in0=iota, scalar=ekall[:, col:col + 1], in1=pstart,
                                                                  op0=ALU.is_equal, op1=ALU.mult, accum_out=dstf[:, col:col + 1]),
                 reads=[drt, dek, dcst], adds=[ddf])
        P.op("dve", lambda e: e.tensor_tensor(out=dstf[:, :], in0=dstf[:, :], in1=slall[:, :], op=ALU.add), reads=[ddf, dek], writes=[ddf, drt])
        P.op("dve", lambda e: e.tensor_copy(K.dst_all[:, :], dstf[:, :]), reads=[drt], adds=[K.ddst])
        for ti in range(32):
            for k in range(4):
                P.dma("pool", lambda e, k=k, ti=ti: e.indirect_dma_start(
                    out=K.Xs_d[:, :], out_offset=bass.IndirectOffsetOnAxis(ap=K.dst_all[:, ti * 4 + k:ti * 4 + k + 1], axis=0),
                    in_=h2all[:, ti, :], in_offset=None), reads=[dh2[ti], K.ddst], adds=[K.dXs])
        for b in range(NBLK):
            P.op("dve", lambda e, b=b: e.scalar_tensor_tensor(out=j32, in0=cb, scalar=float(b), in1=one32, op0=ALU.is_le, op1=ALU.mult,
                                                              accum_out=bexp[:, b:b + 1]), reads=[drt], writes=[drt])
        P.op("dve", lambda e: e.tensor_scalar_mul(bex1024, bexp, 1024.0), reads=[drt], writes=[drt])
        P.op("dve", lambda e: e.tensor_scalar_mul(bex128, bexp, 128.0), reads=[drt], writes=[drt])
        widxf = T("widxf", [128, NBLK * 8], F32)
        for b in range(NBLK):
            P.op("dve", lambda e, b=b: e.tensor_scalar_add(widxf[:, b * 8:(b + 1) * 8], rofs[:, :], bex1024[:, b:b + 1]), reads=[drt, drofs], writes=[drt])
        P.op("dve", lambda e: e.tensor_copy(K.widx[:, :], widxf[:, :]), reads=[drt], adds=[K.dwidx])
        P.op("dve", lambda e: e.tensor_scalar_add(tmp64, bex128, rofs[:, 0:1]), reads=[drt, drofs], writes=[drt])
        P.op("dve", lambda e: e.tensor_copy(K.bgidx[:, :], tmp64[:, 0:NBLK]), reads=[drt], adds=[K.dwidx])
        P.op("dve", lambda e: e.tensor_copy(K.bdidx[:, :], bexp[:, 0:NBLK]), reads=[drt], adds=[K.dwidx])


def moe_T(K, R, x_, dx_):
    P = K.P
    xt_, dxt_ = R["XT"].next()
    for s in range(4):
        pT, dpT = K.pT.next()
        for kc in range(8):
            P.op("pe", lambda e, kc=kc, s=s, pT=pT: e.transpose(pT[:, kc * 128:(kc + 1) * 128], x_[:, s, kc * 128:(kc + 1) * 128], K.ident[:, :]),
                 reads=[dx_, K.dconst], writes=[dpT] if kc == 0 else (), adds=() if kc == 0 else [dpT])
        copy_op(K, _alt(K), xt_[:, :, s * 128:(s + 1) * 128], pT[:, :].rearrange("p (c w) -> p c w", w=128), reads=[dpT],
                writes=[dxt_] if s == 0 else (), adds=[dxt_] if s else ())
    return xt_, dxt_


def moe_gu(K, R, g_, dg_, bgt, dbgt, xt_, dxt_):
    P = K.P
    a_, da_ = R["aT"].next()
    for fc in range(8):
        pg, dpg = K.pF.next()
        pl, dpl = K.pF.next()
        for kc in range(8):
            P.op("pe", lambda e, kc=kc, pg=pg: e.matmul(pg[:, :], lhsT=g_[:, kc, fc * 128:(fc + 1) * 128], rhs=xt_[:, kc, :], start=(kc == 0), stop=(kc == 7)),
                 reads=[dg_, dxt_], writes=[dpg] if kc == 0 else (), adds=() if kc == 0 else [dpg])
        for kc in range(8):
            P.op("pe", lambda e, kc=kc, pl=pl: e.matmul(pl[:, :], lhsT=g_[:, kc, 1024 + fc * 128:1024 + (fc + 1) * 128], rhs=xt_[:, kc, :], start=(kc == 0), stop=(kc == 7)),
                 reads=[dg_, dxt_], writes=[dpl] if kc == 0 else (), adds=() if kc == 0 else [dpl])
        A, dA = R["ta"].next(); S_, dS = R["ts"].next(); L, dL = R["tl"].next()
        P.op("act", lambda e, pg=pg, A=A: e.activation(out=A[:, :], in_=pg[:, :], func=AF.Identity, bias=bgt[:, fc:fc + 1], scale=1.0),
             reads=[dpg, dbgt], writes=[dA])
        P.op("act", lambda e, pl=pl, L=L: e.activation(out=L[:, :], in_=pl[:, :], func=AF.Identity, bias=bgt[:, 8 + fc:9 + fc], scale=1.0),
             reads=[dpl, dbgt], writes=[dL])
        P.op("dve", lambda e, A=A: e.tensor_scalar_min(A[:, :], A[:, :], 7.0), reads=[dA], writes=[dA])
        P.op("act", lambda e, A=A, S_=S_: e.activation(out=S_[:, :], in_=A[:, :], func=AF.Sigmoid, scale=1.702), reads=[dA], writes=[dS])
        P.op("dve", lambda e, L=L: e.tensor_scalar(out=L[:, :], in0=L[:, :], scalar1=7.0, scalar2=-7.0, op0=ALU.min, op1=ALU.max), reads=[dL], writes=[dL])
        P.op("dve", lambda e, A=A, S_=S_: e.tensor_tensor(out=S_[:, :], in0=A[:, :], in1=S_[:, :], op=ALU.mult), reads=[dA, dS], writes=[dS])
        P.op("dve", lambda e, S_=S_, L=L: e.scalar_tensor_tensor(out=a_[:, fc, :], in0=L[:, :], scalar=1.0, in1=S_[:, :], op0=ALU.add, op1=ALU.mult),
             reads=[dS, dL], writes=[da_] if fc == 0 else (), adds=() if fc == 0 else [da_])
    return a_, da_


def moe_down(K, R, d_, dd_, b_, db_, a_, da_, row0):
    P = K.P
    for s in range(4):
        y_, dy_ = R["yo"].next()
        for dh in range(2):
            pd, dpd = K.pF.next()
            for fc in range(8):
                P.op("pe", lambda e, fc=fc, pd=pd: e.matmul(pd[:, :], lhsT=a_[:, fc, s * 128:(s + 1) * 128], rhs=d_[:, fc, dh * 512:(dh + 1) * 512],
                                                            start=(fc == 0), stop=(fc == 7)),
                     reads=[da_, dd_], writes=[dpd] if fc == 0 else (), adds=() if fc == 0 else [dpd])
            P.op("dve", lambda e, pd=pd, dh=dh: e.tensor_tensor(out=y_[:, dh * 512:(dh + 1) * 512], in0=pd[:, :], in1=b_[:, dh * 512:(dh + 1) * 512], op=ALU.add),
                 reads=[dpd, db_], writes=[dy_] if dh == 0 else (), adds=[dy_] if dh else ())
        r0 = row0 + s * 128
        P.dma("sp", lambda e, r0=r0, y_=y_: e.dma_start(out=K.Y_d[r0:r0 + 128, :], in_=y_[:, :]), reads=[dy_], adds=[K.dY])


def phase_MOE(K):
    nc, P = K.nc, K.P
    IO = bass.IndirectOffsetOnAxis
    with ExitStack() as st:
        T = lambda name, shape, dt: st.enter_context(nc.sbuf_tensor("sb_" + name, shape, dt))
        wg = Ring([T("wg%d" % i, [128, 8, 2048], BF16) for i in range(2)])
        wd = Ring([T("wd%d" % i, [128, 8, 1024], BF16) for i in range(2)])
        bdb = Ring([T("bdb%d" % i, [128, 1024], F32) for i in range(2)])
        bgr = Ring([T("bgr%d" % i, [128, 16], F32) for i in range(2)])
        Xe = Ring([T("Xe%d" % i, [128, 4, 1024], BF16) for i in range(2)])
        R = dict(XT=Ring([T("XT%d" % i, [128, 8, 512], BF16) for i in range(2)]),
                 aT=Ring([T("aT%d" % i, [128, 8, 512], BF16) for i in range(2)]),
                 ta=Ring([T("ta%d" % i, [128, 512], F32) for i in range(2)]),
                 ts=Ring([T("ts%d" % i, [128, 512], F32) for i in range(2)]),
                 tl=Ring([T("tl%d" % i, [128, 512], F32) for i in range(2)]),
                 yo=Ring([T("yo%d" % i, [128, 1024], F32) for i in range(2)]))

        def load_w(b):
            g_, dg_ = wg.next()
            for kc in range(8):
                P.dma("pool", lambda e, kc=kc: e.indirect_dma_start(out=g_[:, kc, :], out_offset=None, in_=K.w_gu[:, :],
                                                                    in_offset=IO(ap=K.widx[:, b * 8 + kc:b * 8 + kc + 1], axis=0), bounds_check=K.reg_w, oob_is_err=False),
                      reads=[K.dwidx], writes=[dg_] if kc == 0 else (), adds=[dg_] if kc else ())
            d_, dd_ = wd.next()
            for kc in range(8):
                P.dma("pool", lambda e, kc=kc: e.indirect_dma_start(out=d_[:, kc, :], out_offset=None, in_=K.w_down[:, :],
                                                                    in_offset=IO(ap=K.widx[:, b * 8 + kc:b * 8 + kc + 1], axis=0), bounds_check=K.reg_w, oob_is_err=False),
                      reads=[K.dwidx], writes=[dd_] if kc == 0 else (), adds=[dd_] if kc else ())
            b_, db_ = bdb.next()
            P.dma("pool", lambda e: e.indirect_dma_start(out=b_[:, :], out_offset=None, in_=K.b_down[:, :], in_offset=IO(ap=K.bdidx[:, b:b + 1], axis=0), bounds_check=K.reg_bd, oob_is_err=False),
                  reads=[K.dwidx], writes=[db_])
            t_, dt_ = bgr.next()
            P.dma("pool", lambda e: e.indirect_dma_start(out=t_[:, :], out_offset=None, in_=K.b_gu[:, :], in_offset=IO(ap=K.bgidx[:, b:b + 1], axis=0), bounds_check=K.reg_bg, oob_is_err=False),
                  reads=[K.dwidx], writes=[dt_])
            return g_, dg_, d_, dd_, b_, db_, t_, dt_

        def load_x(bi):
            x_, dx_ = Xe.next()
            P.dma("sp", lambda e: e.dma_start(out=x_[:, :, :], in_=K.Xs_d[bi * 512:(bi + 1) * 512, :].rearrange("(t p) d -> p t d", p=128)),
                  reads=[K.dXs], writes=[dx_])
            return x_, dx_
        W = {0: load_w(0)}
        X = {0: load_x(0)}
        W[1] = load_w(1); X[1] = load_x(1)
        xt = {0: moe_T(K, R, *X[0])}
        aT = {0: moe_gu(K, R, W[0][0], W[0][1], W[0][6], W[0][7], *xt[0])}
        for b in range(NBLK):
            if b + 1 < NBLK:
                xt[b + 1] = moe_T(K, R, *X[b + 1])
            g_, dg_, d_, dd_, b_, db_, t_, dt_ = W[b]
            moe_down(K, R, d_, dd_, b_, db_, *aT[b], b * 512)
            if b + 2 < NBLK:
                W[b + 2] = load_w(b + 2); X[b + 2] = load_x(b + 2)
            if b + 1 < NBLK:
                w1 = W[b + 1]
                aT[b + 1] = moe_gu(K, R, w1[0], w1[1], w1[6], w1[7], *xt[b + 1])


def phase_FINAL(K):
    nc, P = K.nc, K.P
    with ExitStack() as st:
        T = lambda name, shape, dt: st.enter_context(nc.sbuf_tensor("sb_" + name, shape, dt))
        gfin = T("gfin", [128, 1024], F32); dgf = Dep()
        P.dma("sp", lambda e: e.dma_start(out=gfin[:, :], in_=K.gfin[:, :]), writes=[dgf])
        Yk = Ring([T("Yk%d" % i, [128, 1024], F32) for i in range(8)])
        x1 = Ring([T("x1f%d" % i, [128, 1024], F32) for i in range(2)])
        acc = Ring([T("acc%d" % i, [128, 1024], F32) for i in range(2)])
        ob = Ring([T("ob%d" % i, [128, 1, 1024], F32) for i in range(2)])
        stat = Ring([T("stf%d" % i, [128, 2], F32) for i in range(2)])
        for ti in range(32):
            x_, dx_ = x1.next()
            P.dma("sp", lambda e: e.dma_start(out=x_[:, :], in_=K.x1_d[ti * 128:(ti + 1) * 128, :]), reads=[K.dx1], writes=[dx_])
            ys = []
            for k in range(4):
                y_, dy_ = Yk.next()
                P.dma("pool", lambda e, k=k, y_=y_: e.indirect_dma_start(
                    out=y_[:, :], out_offset=None, in_=K.Y_d[:, :], in_offset=bass.IndirectOffsetOnAxis(ap=K.dst_all[:, ti * 4 + k:ti * 4 + k + 1], axis=0)),
                    reads=[K.dY, K.ddst], writes=[dy_])
                ys.append((y_, dy_))
            a_, da_ = acc.next()
            prev, dprev = x_, dx_
            for k in range(4):
                y_, dy_ = ys[k]
                eng = "dve"
                P.op(eng, lambda e, k=k, y_=y_, prev=prev: e.scalar_tensor_tensor(out=a_[:, :], in0=y_[:, :], scalar=K.gates_all[:, ti, k:k + 1], in1=prev[:, :],
                                                                                 op0=ALU.mult, op1=ALU.add),
                     reads=[dy_, dprev, K.dgates], writes=[da_])
                prev, dprev = a_, da_
            s_, ds_ = stat.next()
            P.op("act", lambda e: e.activation(out=K.junk[:, 0:1024], in_=a_[:, :], func=AF.Square, accum_out=s_[:, 0:1]), reads=[da_], writes=[ds_, K.djunk])
            rstd_ops(K, s_, ds_, 1, 1024)
            o_, do_ = ob.next()
            P.op("dve", lambda e: e.scalar_tensor_tensor(out=o_[:, 0, :], in0=a_[:, :], scalar=s_[:, 1:2], in1=gfin[:, :], op0=ALU.mult, op1=ALU.mult),
                 reads=[da_, ds_, dgf], writes=[do_])
            P.dma("sp", lambda e: e.dma_start(out=K.out[ti * 128:(ti + 1) * 128, :], in_=o_[:, 0, :]), reads=[do_], adds=[K.dout])


def build(stop_after=None, lite=False):
    nc = bass.Bass("TRN2", target_bir_lowering=False)
    K = Ctx(); K.nc = nc; K.alt = 0
    ein = lambda name, shape, dt=F32: nc.dram_tensor(name, shape, dt, kind="ExternalInput").ap()
    scr = lambda name, shape, dt: nc.dram_tensor(name, shape, dt, kind="ExternalOutput" if DEBUG else "Internal").ap()
    K.x_core = ein("x_core", [SEQ, D]); K.x_kv = ein("x_kv", [KVTOK, D]); K.mem = ein("mem", [256, D])
    K.gmix = ein("gmix", [128, D]); K.gmem = ein("gmem", [128, D]); K.ggrp = ein("ggrp", [128, 1536])
    K.gffn = ein("gffn", [128, D]); K.gfin = ein("gfin", [128, D]); K.router_b = ein("router_b", [128, NEXP])
    K.w_in = ein("w_in", [D, 2560]); K.w_kv = ein("w_kv", [D, 1024]); K.w_out = ein("w_out", [1536, D])
    ne = 1 if lite else NEXP
    K.router_w = ein("router_w", [D, NEXP]); K.w_gu = ein("w_gu", [ne * D, 2048]); K.b_gu = ein("b_gu", [NEXP * 128, 16])
    K.w_down = ein("w_down", [ne * D, D]); K.b_down = ein("b_down", [NEXP, D]); K.rowoff = ein("rowoff", [128, 8])
    K.na_bias = ein("na_bias", [9, 128, 2048]); K.na_mask = ein("na_mask", [128, 2048], BF16)
    K.cs4 = ein("cs4", [128, 512], BF16); K.etw = ein("etw", [128, 64, 2, 128], BF16); K.dmat = ein("dmat", [128, 32], BF16)
    K.identd = ein("ident", [128, 128], BF16); K.lo = ein("lo", [128, 2, 128], BF16); K.iota = ein("iota", [128, NEXP])
    K.out = nc.dram_tensor("out", [TOK, D], F32, kind="ExternalOutput").ap()
    K.qT_d = scr("qT_d", [512, TOK], BF16); K.kT_d = scr("kT_d", [512, KVTOK], BF16); K.V_d = scr("V_d", [KVTOK, 520], BF16)
    K.uT_d = scr("uT_d", [512, SEQ], BF16); K.qmT_d = scr("qmT_d", [512, TOK], BF16); K.yT_d = scr("yT_d", [1536, TOK], BF16)
    K.B_d = scr("B_d", [4, 2, 64, 128, 128], BF16); K.x1_d = scr("x1_d", [TOK, D], F32)
    K.Xs_d = scr("Xs_d", [NBLK * BLK, D], BF16); K.Y_d = scr("Y_d", [NBLK * BLK, D], F32)
    for n in ("dqT", "dkT", "dV", "duT", "dqmT", "dyT", "dB", "dx1", "dXs", "dY", "dout", "ddst", "dgates", "dconst", "djunk", "dwidx"):
        setattr(K, n, Dep())
    with ExitStack() as st:
        P = Prog(nc, st); K.P = P
        T = lambda name, shape, dt: st.enter_context(nc.sbuf_tensor("sb_" + name, shape, dt))
        K.ident = T("ident_sb", [128, 128], BF16)
        P.dma("sp", lambda e: e.dma_start(out=K.ident[:, :], in_=K.identd[:, :]), writes=[K.dconst])
        K.junk = T("junk", [128, 1024], F32)
        K.stat = Ring([T("stat%d" % i, [128, 8], F32) for i in range(4)])
        K.gates_all = T("gates_all", [128, 32, 4], F32); K.dst_all = T("dst_all", [128, 128], U32)
        K.widx = T("widx", [128, NBLK * 8], U32); K.bgidx = T("bgidx", [128, NBLK], U32); K.bdidx = T("bdidx", [128, NBLK], U32)
        K.reg_w = nc.gpsimd.to_reg(NEXP * D - 1); K.reg_bd = nc.gpsimd.to_reg(NEXP - 1); K.reg_bg = nc.gpsimd.to_reg(NEXP * 128 - 1)
        zt = T("zt", [128, 4, 1024], BF16); dzt = Dep()
        phases = [("A", phase_A), ("MEM", phase_MEM), ("NA", phase_NA), ("FT", phase_FT), ("OUT", phase_OUT), ("MOE", phase_MOE), ("FINAL", phase_FINAL)]
        for name, fn in phases:
            npt = 4 if name in ("A", "MOE") else 2
            with ExitStack() as pst:
                K.pT = Ring([pst.enter_context(nc.psum_tensor("pT_%s%d" % (name, i), [128, 1024], BF16)) for i in range(npt)], psum=True)
                K.pF = Ring([pst.enter_context(nc.psum_tensor("pF_%s%d" % (name, i), [128, 512], F32)) for i in range(8 - npt)], psum=True)
                fn(K)
                P.barrier()
            if name == "MEM":
                P.op("pool", lambda e: e.memset(zt[:, :, :], 0.0), writes=[dzt])
                for zi in range(NBLK):
                    P.dma("pool", lambda e, zi=zi: e.dma_start(out=K.Xs_d[zi * 512:(zi + 1) * 512, :].rearrange("(t p) d -> p t d", p=128), in_=zt[:, :, :]),
                          reads=[dzt], adds=[K.dXs])
            if stop_after == name:
                break
        if DEBUG:
            for nm, til, dt_, w_ in (("dbg_dst", K.dst_all, U32, 128), ("dbg_widx", K.widx, U32, NBLK * 8), ("dbg_bdidx", K.bdidx, U32, NBLK), ("dbg_bgidx", K.bgidx, U32, NBLK)):
                dd = nc.dram_tensor(nm, [128, w_], dt_, kind="ExternalOutput").ap()
                P.dma("sp", lambda e, dd=dd, til=til: e.dma_start(out=dd[:, :], in_=til[:, :]), adds=[K.dout])
            dd = nc.dram_tensor("dbg_gates", [128, 128], F32, kind="ExternalOutput").ap()
            P.dma("sp", lambda e, dd=dd: e.dma_start(out=dd[:, :], in_=K.gates_all[:, :, :].rearrange("p a b -> p (a b)")), adds=[K.dout])
        if stop_after is not None and stop_after != "FINAL":
            P.dma("sp", lambda e: e.dma_start(out=K.out[0:128, :], in_=K.junk[:, :]), adds=[K.dout])
        P.drain("sp")
        K.ninst = P.ninst
    return nc, K


def _consts():
    bf = ml_dtypes.bfloat16
    c = np.arange(128, dtype=np.float64)
    ang = 2 * np.pi * np.outer(c, c) / 128.0
    C, S = np.cos(ang), np.sin(ang)
    cs4 = np.concatenate([C, -S, -S, -C], axis=1).astype(bf)
    s2 = np.arange(128, dtype=np.float64)[:, None, None]
    s1 = np.arange(64, dtype=np.float64)[None, :, None]
    k2 = np.arange(128, dtype=np.float64)[None, None, :]
    th = 2 * np.pi * k2 * (s1 + 64 * s2) / 8192.0
    et = np.stack([np.cos(th), np.sin(th)], axis=2)
    sign = np.where(np.arange(128) % 2 == 0, 1.0, -1.0)[None, None, None, :]
    etw = [et.astype(bf), (et * sign).astype(bf)]
    dm = []
    s1v = np.arange(64, dtype=np.float64)[:, None]
    for hf in range(2):
        k1 = (hf * 32 + np.arange(32, dtype=np.float64))[None, :]
        ph = 2 * np.pi * k1 * s1v / 64.0
        dm.append((np.concatenate([np.cos(ph), np.sin(ph)], axis=0) * 2.0 ** -10).astype(bf))
    ident = np.eye(128).astype(bf)
    lo = np.stack([np.triu(np.ones((128, 128)), 1), np.ones((128, 128))], axis=1).astype(bf)
    iota = np.broadcast_to(np.arange(NEXP, dtype=np.float32)[None, :], (128, NEXP)).copy()
    cq = np.arange(64)
    col_start = np.clip(cq - 8, 0, 64 - 16)
    col_in = (cq[None, :] >= col_start[:, None]) & (cq[None, :] < col_start[:, None] + 16)
    mask = np.broadcast_to(col_in.T[None, :, None, None, :], (2, 64, 8, 4, 64)).reshape(128, 2048).astype(bf)
    rowoff = (np.arange(8, dtype=np.float32)[None, :] * 128 + np.arange(128, dtype=np.float32)[:, None]).astype(np.float32)
    return dict(cs4=cs4, etw=etw, dmat=dm, ident=ident, lo=lo, iota=iota, na_mask=mask, rowoff=rowoff)


def _kv_rows(hf):
    r0 = 64 * hf - 4
    rows = []
    for j in range(KVROWS):
        r = r0 + j
        if r < 0:
            r = 4 + j
        if r > 127:
            r = 120 + (r - 128)
        rows.append(r)
    return np.array(rows)


def _na_bias_table(rel_bias, hf):
    rows = _kv_rows(hf)
    ck = np.arange(64)[:, None]; cq = np.arange(64)[None, :]
    dc = np.clip(ck - cq, -15, 15) + 15
    out = np.empty((64, 64, 8, 8, 64), np.float32)
    for lq in range(64):
        r = 64 * hf + lq
        for j in range(8):
            dr = int(rows[lq + j] - r)
            assert -7 <= dr <= 7
            out[lq, :, :, j, :] = rel_bias[:, dr + 7, :][:, dc].transpose(1, 0, 2)
    sel = [lq for lq in range(64) if (lq <= 4 or lq >= 60)]
    o2 = out[sel][:, :, HORD].reshape(len(sel), 64, 8, 4, 2, 64).transpose(0, 4, 1, 2, 3, 5)
    return np.ascontiguousarray(o2).reshape(len(sel), 128, 2048)


def make_in_maps(inputs):
    f = lambda a: np.ascontiguousarray(np.asarray(a, dtype=np.float32))
    x = f(inputs["x"]); mem = f(inputs["mem"])
    cst = _consts()
    bc = lambda v: np.ascontiguousarray(np.broadcast_to(f(v).reshape(1, -1), (128, f(v).size)))
    shared = dict(
        gmix=bc(inputs["g_mix"][0]), gmem=bc(inputs["g_mem"][0]), ggrp=bc(inputs["g_grp"][0]), gffn=bc(inputs["g_ffn"][0]),
        gfin=bc(inputs["g_final"]), router_b=bc(inputs["router_b"][0]),
        w_in=f(inputs["w_in"][0]), w_kv=f(inputs["w_mem_kv"][0]), w_out=f(inputs["w_out"][0]), router_w=f(inputs["router_w"][0]),
        w_gu=f(inputs["w_gu"][0]).reshape(NEXP * D, 2048), w_down=f(inputs["w_down"][0]).reshape(NEXP * D, D),
        b_gu=np.ascontiguousarray(f(inputs["b_gu"][0]).reshape(NEXP, 16, 128).transpose(0, 2, 1)).reshape(NEXP * 128, 16),
        b_down=f(inputs["b_down"][0]), rowoff=cst["rowoff"],
        na_mask=cst["na_mask"], cs4=cst["cs4"], ident=cst["ident"], lo=cst["lo"], iota=cst["iota"],
    )
    rel = f(inputs["na_rel_bias"][0])
    tabs = [_na_bias_table(rel, hf) for hf in range(2)]
    maps = []
    for c in range(NCORES):
        b, hf = c // 2, c % 2
        xb = x[b]
        xc = np.concatenate([xb[hf * TOK:(hf + 1) * TOK], xb[(1 - hf) * TOK:(2 - hf) * TOK]], axis=0)
        rows = _kv_rows(hf)
        xkv = xb.reshape(128, 64, D)[rows].reshape(KVTOK, D)
        m = dict(shared)
        m.update(x_core=np.ascontiguousarray(xc), x_kv=np.ascontiguousarray(xkv), mem=mem[b], na_bias=tabs[hf],
                 etw=cst["etw"][hf], dmat=cst["dmat"][hf])
        maps.append(m)
    return maps


def kernel(**inputs):
    nc, K = build()
    maps = make_in_maps(inputs)
    res = run_bass_kernel_spmd(nc, maps, core_ids=list(range(NCORES)))
    out = np.empty((4, SEQ, D), np.float32)
    for c in range(NCORES):
        b, hf = c // 2, c % 2
        out[b, hf * TOK:(hf + 1) * TOK] = res.results[c]["out"]
    return out
```

```python
import os
import numpy as np
import ml_dtypes
import concourse.bass as bass
import concourse.mybir as mybir
from concourse.bass_utils import run_bass_kernel_spmd
from contextlib import ExitStack

F32 = mybir.dt.float32; BF16 = mybir.dt.bfloat16; U32 = mybir.dt.uint32
AF = mybir.ActivationFunctionType; ALU = mybir.AluOpType

NCORES = 8
D = 1024
SEQ = 8192
TOK = 4096
KVROWS = 72
KVTOK = KVROWS * 64
NEXP = 32
BLK = 512
HORD = [0, 2, 1, 3, 4, 6, 5, 7]
NBLK = 63
DEBUG = bool(int(os.environ.get("MK_DEBUG", "0")))
CUT = int(os.environ.get("MK_CUT", "0"))


class Dep:
    __slots__ = ("w", "r", "prev", "psum")
    def __init__(self, psum=False):
        self.w = []; self.r = []; self.prev = []; self.psum = psum


class Prog:
    NCH = {"sp": 3, "pool": 5}

    def __init__(self, nc, stack):
        self.nc = nc; self.stack = stack
        self.eng = {"pe": nc.tensor, "act": nc.scalar, "dve": nc.vector, "pool": nc.gpsimd, "sp": nc.sync}
        self.sem = {e: stack.enter_context(nc.semaphore("s_" + e)) for e in self.eng}
        self.count = {e: 0 for e in self.eng}
        self.waited = {e: {} for e in self.eng}
        self.chan = {}
        self.rr = {}
        self.ninst = 0

    def _sem(self, key):
        return self.sem[key] if key in self.sem else self.chan[key][0]

    def _wait(self, eng, events):
        best = {}
        for (k, v) in events:
            if k == eng and eng == "pe":
                continue
            if best.get(k, 0) < v:
                best[k] = v
        wd = self.waited[eng]
        for k, v in best.items():
            if wd.get(k, 0) >= v:
                continue
            wd[k] = v
            self.eng[eng].wait_ge(self._sem(k), v)

    @staticmethod
    def _collect(reads, writes, adds, eng=None):
        ev = []
        for d in reads:
            ev += d.w
            if d.psum:
                ev += [x for x in d.r if x[0] != eng]
        for d in writes:
            ev += d.w; ev += d.r
        for d in adds:
            ev += d.r; ev += d.prev
        return ev

    @staticmethod
    def _update(me, reads, writes, adds):
        for d in reads:
            d.r.append(me)
            if len(d.r) > 64:
                d.r = Prog._compress(d.r)
        for d in writes:
            d.prev = Prog._compress(d.w + d.r)
            d.w = [me]; d.r = []
        for d in adds:
            d.w.append(me)
            if len(d.w) > 64:
                d.w = Prog._compress(d.w)

    @staticmethod
    def _compress(evs):
        best = {}
        for k, v in evs:
            if best.get(k, 0) < v:
                best[k] = v
        return list(best.items())

    def op(self, eng, fn, reads=(), writes=(), adds=()):
        self._wait(eng, self._collect(reads, writes, adds, eng))
        self.count[eng] += 1
        fn(self.eng[eng]).then_inc(self.sem[eng], 1)
        self._update((eng, self.count[eng]), reads, writes, adds)
        self.ninst += 1

    def dma(self, q, fn, reads=(), writes=(), adds=()):
        self.rr[q] = (self.rr.get(q, -1) + 1) % self.NCH[q]
        chan = "%s%d" % (q, self.rr[q])
        if chan not in self.chan:
            self.chan[chan] = [self.stack.enter_context(self.nc.semaphore("c_" + chan)), 0]
        c = self.chan[chan]
        ev = self._collect(reads, writes, adds)
        if c[1] > 0:
            ev.append((chan, 16 * c[1]))
        self._wait(q, ev)
        c[1] += 1
        fn(self.eng[q]).then_inc(c[0], 16)
        self._update((chan, 16 * c[1]), reads, writes, adds)
        self.ninst += 1

    def barrier(self):
        ev = [(e, n) for e, n in self.count.items() if n > 0]
        ev += [(ch, 16 * c[1]) for ch, c in self.chan.items() if c[1] > 0]
        for e in self.eng:
            self._wait(e, ev)

    def drain(self, eng="sp"):
        ev = [(e, n) for e, n in self.count.items() if n > 0]
        ev += [(ch, 16 * c[1]) for ch, c in self.chan.items() if c[1] > 0]
        self._wait(eng, ev)


class Ring:
    def __init__(self, items, psum=False):
        self.items = items; self.deps = [Dep(psum) for _ in items]; self.i = -1
    def next(self):
        self.i = (self.i + 1) % len(self.items)
        return self.items[self.i], self.deps[self.i]


class Ctx:
    pass


def _alt(K):
    K.alt ^= 1
    return "act" if K.alt else "dve"


def copy_op(K, eng, out, in_, reads, writes=(), adds=()):
    if eng == "act":
        K.P.op("act", lambda e: e.copy(out, in_), reads=reads, writes=writes, adds=adds)
    else:
        K.P.op(eng, lambda e: e.tensor_copy(out, in_), reads=reads, writes=writes, adds=adds)


def rstd_ops(K, stat, dstat, n, width):
    P = K.P
    P.op("dve", lambda e: e.tensor_scalar(out=stat[:, n:2 * n], in0=stat[:, 0:n], scalar1=1.0 / width, scalar2=1e-6,
                                          op0=ALU.mult, op1=ALU.add), reads=[dstat], writes=[dstat])
    P.op("act", lambda e: e.sqrt(stat[:, n:2 * n], stat[:, n:2 * n]), reads=[dstat], writes=[dstat])
    P.op("dve", lambda e: e.reciprocal(stat[:, n:2 * n], stat[:, n:2 * n]), reads=[dstat], writes=[dstat])


def norm_tiles(K, srcs, dsrc, outb, doutb, g_ap, dg, width, np_=128):
    P = K.P
    n = len(srcs)
    stat, dstat = K.stat.next()
    for t, s in enumerate(srcs):
        P.op("act", lambda e, s=s, t=t: e.activation(out=K.junk[0:np_, 0:width], in_=s, func=AF.Square,
                                                      accum_out=stat[0:np_, t:t + 1]),
             reads=[dsrc], writes=[dstat, K.djunk] if t == 0 else [K.djunk], adds=[dstat] if t else ())
    P.op("dve", lambda e: e.tensor_scalar(out=stat[0:np_, n:2 * n], in0=stat[0:np_, 0:n], scalar1=1.0 / width, scalar2=1e-6,
                                          op0=ALU.mult, op1=ALU.add), reads=[dstat], writes=[dstat])
    P.op("act", lambda e: e.sqrt(stat[0:np_, n:2 * n], stat[0:np_, n:2 * n]), reads=[dstat], writes=[dstat])
    P.op("dve", lambda e: e.reciprocal(stat[0:np_, n:2 * n], stat[0:np_, n:2 * n]), reads=[dstat], writes=[dstat])
    for t, s in enumerate(srcs):
        P.op("dve", lambda e, s=s, t=t: e.scalar_tensor_tensor(out=outb[0:np_, t, :], in0=s, scalar=stat[0:np_, n + t:n + t + 1],
                                                               in1=g_ap[0:np_, :], op0=ALU.mult, op1=ALU.mult),
             reads=[dsrc, dstat, dg], writes=[doutb] if t == 0 else (), adds=[doutb] if t else ())


def transpose_block(K, inb, dinb, ntile, nchunk, out3, dout, np_=128, col0=0):
    P = K.P
    per = 1024 // (ntile * np_)
    first = True
    for c0 in range(0, nchunk, per):
        pT, dpT = K.pT.next()
        cs = range(c0, min(nchunk, c0 + per))
        for j, c in enumerate(cs):
            for t in range(ntile):
                o = (j * ntile + t) * np_
                P.op("pe", lambda e, o=o, c=c, t=t: e.transpose(pT[:, o:o + np_], inb[0:np_, t, c * 128:(c + 1) * 128],
                                                                K.ident[0:np_, 0:np_]),
                     reads=[dinb, K.dconst], writes=[dpT] if (j == 0 and t == 0) else (),
                     adds=() if (j == 0 and t == 0) else [dpT])
        w = ntile * np_
        nn = len(cs)
        copy_op(K, _alt(K), out3[:, c0:c0 + nn, col0:col0 + w],
                pT[:, 0:nn * w].rearrange("p (c w) -> p c w", w=w), reads=[dpT],
                writes=[dout] if first else (), adds=() if first else [dout])
        first = False


def emit_y(K, srcs, dsrc, g_ap, dg, row0, tok0):
    P = K.P
    yb, dyb = K.yb.next()
    norm_tiles(K, srcs, dsrc, yb, dyb, g_ap, dg, 512)
    ys, dys = K.ystage.next()
    transpose_block(K, yb, dyb, 4, 4, ys, dys)
    P.dma("pool", lambda e: e.dma_start(out=K.yT_d[row0:row0 + 512, tok0:tok0 + 512].rearrange("(c p) t -> p c t", p=128),
                                        in_=ys[:, :, :]), reads=[dys], adds=[K.dyT])


def phase_A(K):
    nc, P = K.nc, K.P
    with ExitStack() as st:
        T = lambda name, shape, dt: st.enter_context(nc.sbuf_tensor("sb_" + name, shape, dt))
        w = T("w_in", [128, 8, 2560], BF16); dw = Dep()
        for kc in range(8):
            for h2 in range(2):
                P.dma("pool", lambda e, kc=kc, h2=h2: e.dma_start(out=w[:, kc, h2 * 1280:(h2 + 1) * 1280],
                                                                  in_=K.w_in[kc * 128:(kc + 1) * 128, h2 * 1280:(h2 + 1) * 1280]),
                      adds=[dw])
        gm = T("gmix", [128, 1024], F32); dgm = Dep()
        P.dma("sp", lambda e: e.dma_start(out=gm[:, :], in_=K.gmix[:, :]), writes=[dgm])
        xr = Ring([T("xr%d" % i, [128, 4, 1024], F32) for i in range(2)])
        hb = Ring([T("hb%d" % i, [128, 4, 1024], BF16) for i in range(2)])
        hT = Ring([T("hT%d" % i, [128, 8, 512], BF16) for i in range(2)])
        stg = Ring([T("stg%d" % i, [128, 512], BF16) for i in range(4)])
        vst = Ring([T("vst%d" % i, [128, 8, 65], BF16) for i in range(2)])
        for v, dv in zip(vst.items, vst.deps):
            P.op("pool", lambda e, v=v: e.memset(v[:, :, :], 1.0), writes=[dv])
        blocks = [("core", i) for i in range(16)] + [("kv", i) for i in range(KVTOK // 512)]

        def load(bi):
            kind, i = blocks[bi]
            src = K.x_core if kind == "core" else K.x_kv
            x_, dx_ = xr.next()
            P.dma("sp", lambda e: e.dma_start(out=x_[:, :, :], in_=src[i * 512:(i + 1) * 512, :].rearrange("(t p) d -> p t d", p=128)),
                  writes=[dx_])
            return x_, dx_
        def norm_steps(xd):
            x_, dx_ = xd
            h_, dh_ = hb.next()
            stat, dstat = K.stat.next()
            steps = []
            for t in range(4):
                steps.append(lambda t=t: P.op("act", lambda e: e.activation(out=K.junk[:, 0:1024], in_=x_[:, t, :], func=AF.Square, accum_out=stat[:, t:t + 1]),
                                              reads=[dx_], writes=[dstat, K.djunk] if t == 0 else [K.djunk], adds=[dstat] if t else ()))

            def rstd():
                P.op("dve", lambda e: e.tensor_scalar(out=stat[:, 4:8], in0=stat[:, 0:4], scalar1=1.0 / 1024, scalar2=1e-6, op0=ALU.mult, op1=ALU.add),
                     reads=[dstat], writes=[dstat])
                P.op("act", lambda e: e.sqrt(stat[:, 4:8], stat[:, 4:8]), reads=[dstat], writes=[dstat])
                P.op("dve", lambda e: e.reciprocal(stat[:, 4:8], stat[:, 4:8]), reads=[dstat], writes=[dstat])
            steps.append(rstd)
            for t in range(4):
                steps.append(lambda t=t: P.op("dve", lambda e: e.scalar_tensor_tensor(out=h_[:, t, :], in0=x_[:, t, :], scalar=stat[:, 4 + t:5 + t], in1=gm[:, :],
                                                                                     op0=ALU.mult, op1=ALU.mult),
                                              reads=[dx_, dstat, dgm], writes=[dh_] if t == 0 else (), adds=[dh_] if t else ()))
            return (h_, dh_), steps
        X = {0: load(0)}
        if len(blocks) > 1:
            X[1] = load(1)
        H = {}
        H[0], st0 = norm_steps(X[0])
        for f_ in st0:
            f_()
        for bi, (kind, i) in enumerate(blocks):
            h_, dh_ = H[bi]
            hT_, dhT_ = hT.next()
            transpose_block(K, h_, dh_, 4, 8, hT_, dhT_)
            pend = []
            if bi + 1 < len(blocks):
                H[bi + 1], pend = norm_steps(X[bi + 1])
            if bi + 2 < len(blocks):
                X[bi + 2] = load(bi + 2)
            if kind == "core":
                fcs = [12, 13, 14, 15]
                if i < 8:
                    fcs = [0, 1, 2, 3, 16, 17, 18, 19] + fcs
            else:
                fcs = [4, 5, 6, 7]
            for fc in fcs:
                pf, dpf = K.pF.next()
                for kc in range(8):
                    P.op("pe", lambda e, kc=kc, fc=fc, pf=pf: e.matmul(pf[:, :], lhsT=w[:, kc, fc * 128:(fc + 1) * 128], rhs=hT_[:, kc, :],
                                                                       start=(kc == 0), stop=(kc == 7)),
                         reads=[dw, dhT_], writes=[dpf] if kc == 0 else (), adds=() if kc == 0 else [dpf])
                s_, ds_ = stg.next()
                copy_op(K, _alt(K), s_[:, :], pf[:, :], reads=[dpf], writes=[ds_])
                if fc < 4:
                    dst, dd = K.qT_d[fc * 128:(fc + 1) * 128, i * 512:(i + 1) * 512], K.dqT
                elif fc < 8:
                    dst, dd = K.kT_d[(fc - 4) * 128:(fc - 3) * 128, i * 512:(i + 1) * 512], K.dkT
                elif fc < 16:
                    dst, dd = K.uT_d[(fc - 12) * 128:(fc - 11) * 128, i * 512:(i + 1) * 512], K.duT
                else:
                    dst, dd = K.qmT_d[(fc - 16) * 128:(fc - 15) * 128, i * 512:(i + 1) * 512], K.dqmT
                P.dma("pool", lambda e, dst=dst, s_=s_: e.dma_start(out=dst, in_=s_[:, :]), reads=[ds_], adds=[dd])
                for _ in range(-(-9 // max(1, len(fcs)))):
                    if pend:
                        pend.pop(0)()
            if kind == "kv":
                for t in range(4):
                    pf, dpf = K.pF.next()
                    for kc in range(8):
                        P.op("pe", lambda e, kc=kc, t=t, pf=pf: e.matmul(pf[:, :], lhsT=hT_[:, kc, t * 128:(t + 1) * 128], rhs=w[:, kc, 1024:1536],
                                                                         start=(kc == 0), stop=(kc == 7)),
                             reads=[dw, dhT_], writes=[dpf] if kc == 0 else (), adds=() if kc == 0 else [dpf])
                    v_, dv_ = vst.next()
                    copy_op(K, _alt(K), v_[:, :, 0:64], pf[:, :].rearrange("p (h d) -> p h d", d=64), reads=[dpf], writes=[dv_])
                    r0 = i * 512 + t * 128
                    P.dma("pool", lambda e, r0=r0, v_=v_: e.dma_start(out=K.V_d[r0:r0 + 128, :], in_=v_[:, :, :].rearrange("p h d -> p (h d)")),
                          reads=[dv_], adds=[K.dV])
            while pend:
                pend.pop(0)()


def phase_MEM(K):
    nc, P = K.nc, K.P
    with ExitStack() as st:
        T = lambda name, shape, dt: st.enter_context(nc.sbuf_tensor("sb_" + name, shape, dt))
        K.yb = Ring([T("ybm%d" % i, [128, 4, 512], BF16) for i in range(2)])
        K.ystage = Ring([T("ystm%d" % i, [128, 4, 512], BF16) for i in range(2)])
        wkv = T("wkv", [128, 8, 1024], BF16); dwkv = Dep()
        for kc in range(8):
            P.dma("pool", lambda e, kc=kc: e.dma_start(out=wkv[:, kc, :], in_=K.w_kv[kc * 128:(kc + 1) * 128, :]), adds=[dwkv])
        gme = T("gmem", [128, 1024], F32); dgme = Dep()
        P.dma("sp", lambda e: e.dma_start(out=gme[:, :], in_=K.gmem[:, :]), writes=[dgme])
        gg = T("ggm", [128, 512], F32); dgg = Dep()
        P.dma("sp", lambda e: e.dma_start(out=gg[:, :], in_=K.ggrp[:, 1024:1536]), writes=[dgg])
        mx = T("memx", [128, 2, 1024], F32); dmx = Dep()
        P.dma("sp", lambda e: e.dma_start(out=mx[:, :, :], in_=K.mem[:, :].rearrange("(t p) d -> p t d", p=128)), writes=[dmx])
        mb = T("memb", [128, 2, 1024], BF16); dmb = Dep()
        norm_tiles(K, [mx[:, t, :] for t in range(2)], dmx, mb, dmb, gme, dgme, 1024)
        memT = T("memT", [128, 8, 256], BF16); dmemT = Dep()
        transpose_block(K, mb, dmb, 2, 8, memT, dmemT)
        if CUT == 1: return
        kmT = T("kmT", [128, 4, 256], BF16); dkmT = Dep()
        for h in range(4):
            pf, dpf = K.pF.next()
            for kc in range(8):
                P.op("pe", lambda e, kc=kc, h=h, pf=pf: e.matmul(pf[:, 0:256], lhsT=wkv[:, kc, h * 128:(h + 1) * 128], rhs=memT[:, kc, :],
                                                                 start=(kc == 0), stop=(kc == 7)),
                     reads=[dwkv, dmemT], writes=[dpf] if kc == 0 else (), adds=() if kc == 0 else [dpf])
            copy_op(K, _alt(K), kmT[:, h, :], pf[:, 0:256], reads=[dpf], writes=[dkmT] if h == 0 else (), adds=[dkmT] if h else ())
        if CUT == 2: return
        Vm = T("Vm", [128, 2, 4, 129], BF16); dVm = Dep()
        P.op("pool", lambda e: e.memset(Vm[:, :, :, :], 1.0), writes=[dVm])
        for mt in range(2):
            pf, dpf = K.pF.next()
            for kc in range(8):
                P.op("pe", lambda e, kc=kc, mt=mt, pf=pf: e.matmul(pf[:, :], lhsT=memT[:, kc, mt * 128:(mt + 1) * 128], rhs=wkv[:, kc, 512:1024],
                                                                   start=(kc == 0), stop=(kc == 7)),
                     reads=[dwkv, dmemT], writes=[dpf] if kc == 0 else (), adds=() if kc == 0 else [dpf])
            copy_op(K, _alt(K), Vm[:, mt, :, 0:128], pf[:, :].rearrange("p (h d) -> p h d", d=128), reads=[dpf, dVm], writes=[dVm])
        if CUT == 3: return
        qm = Ring([T("qm%d" % i, [128, 4, 512], BF16) for i in range(2)])
        PT = Ring([T("PTm%d" % i, [128, 2, 512], BF16) for i in range(2)])
        ym = Ring([T("ym%d" % i, [128, 4, 512], F32) for i in range(2)])
        rd = Ring([T("rdm%d" % i, [128, 4], F32) for i in range(4)])
        def load_qm(blk):
            q_, dq_ = qm.next()
            P.dma("sp", lambda e: e.dma_start(out=q_[:, :, :], in_=K.qmT_d[:, blk * 512:(blk + 1) * 512].rearrange("(h p) t -> p h t", p=128)),
                  reads=[K.dqmT], writes=[dq_])
            return q_, dq_
        nq = load_qm(0)
        for blk in range(8):
            q_, dq_ = nq
            if blk < 7:
                nq = load_qm(blk + 1)
            y_, dy_ = ym.next()

            def s1(h):
                pt_, dpt_ = PT.next()
                for mt in range(2):
                    pf, dpf = K.pF.next()
                    P.op("pe", lambda e, mt=mt, pf=pf: e.matmul(pf[:, :], lhsT=kmT[:, h, mt * 128:(mt + 1) * 128], rhs=q_[:, h, :], start=True, stop=True),
                         reads=[dkmT, dq_], writes=[dpf])
                    P.op("act", lambda e, mt=mt, pf=pf: e.activation(out=pt_[:, mt, :], in_=pf[:, :], func=AF.Exp, scale=float(128 ** -0.5)),
                         reads=[dpf], writes=[dpt_] if mt == 0 else (), adds=[dpt_] if mt else ())
                return pt_, dpt_

            def s2(h, pt_, dpt_):
                pa, dpa = K.pF.next()
                pb, dpb = K.pF.next()
                for t in range(4):
                    po = pa[:, t * 129:(t + 1) * 129] if t < 3 else pb[:, 0:129]
                    dpo = dpa if t < 3 else dpb
                    for mt in range(2):
                        P.op("pe", lambda e, mt=mt, t=t, po=po: e.matmul(po, lhsT=pt_[:, mt, t * 128:(t + 1) * 128], rhs=Vm[:, mt, h, :],
                                                                         start=(mt == 0), stop=(mt == 1)),
                             reads=[dpt_, dVm], writes=[dpo] if (mt == 0 and t in (0, 3)) else (),
                             adds=() if (mt == 0 and t in (0, 3)) else [dpo])
                r_, dr_ = rd.next()
                P.op("dve", lambda e: e.reciprocal(r_[:, 0:3], pa[:, 0:387].rearrange("p (t c) -> p t c", c=129)[:, :, 128]),
                     reads=[dpa], writes=[dr_])
                P.op("dve", lambda e: e.reciprocal(r_[:, 3:4], pb[:, 128:129]), reads=[dpb], adds=[dr_])
                for t in range(4):
                    po = pa[:, t * 129:t * 129 + 128] if t < 3 else pb[:, 0:128]
                    dpo = dpa if t < 3 else dpb
                    if t < 3:
                        P.op("act", lambda e, t=t, po=po: e.mul(y_[:, t, h * 128:(h + 1) * 128], po, r_[:, t:t + 1]),
                             reads=[dpo, dr_], writes=[dy_] if (h == 0 and t == 0) else (), adds=() if (h == 0 and t == 0) else [dy_])
                    else:
                        P.op("dve", lambda e, t=t, po=po: e.tensor_scalar_mul(y_[:, t, h * 128:(h + 1) * 128], po, r_[:, t:t + 1]),
                             reads=[dpo, dr_], adds=[dy_])
            cur = s1(0)
            for h in range(4):
                nxt_ = s1(h + 1) if h < 3 else None
                s2(h, *cur)
                cur = nxt_
            if CUT == 6: return
            emit_y(K, [y_[:, t, :] for t in range(4)], dy_, gg, dgg, 1024, blk * 512)
            if CUT == 7: return


def phase_NA(K):
    nc, P = K.nc, K.P
    with ExitStack() as st:
        T = lambda name, shape, dt: st.enter_context(nc.sbuf_tensor("sb_" + name, shape, dt))
        qTr = Ring([T("qT%d" % i, [128, 4, 512], BF16) for i in range(2)])
        kT = T("kT", [128, 4, KVTOK], BF16); dk = Dep()
        for fc in range(4):
            P.dma("sp", lambda e, fc=fc: e.dma_start(out=kT[:, fc, :], in_=K.kT_d[fc * 128:(fc + 1) * 128, :]), reads=[K.dkT], adds=[dk])
        pO = Ring([(K.pF.items[0], K.pF.items[1])])
        pO.deps = [(K.pF.deps[0], K.pF.deps[1])]
        pS = Ring([K.pF.items[i] for i in (2, 3, 4, 5)]); pS.deps = [K.pF.deps[i] for i in (2, 3, 4, 5)]
        maskf = T("maskf", [128, 2048], BF16); dmask = Dep()
        P.dma("sp", lambda e: e.dma_start(out=maskf[:, :], in_=K.na_mask[:, :]), writes=[dmask])
        gg = T("ggn", [64, 512], F32); dgg = Dep()
        P.dma("sp", lambda e: e.dma_start(out=gg[:, :], in_=K.ggrp[0:64, 0:512]), writes=[dgg])
        Vp = [T("Vp%d" % i, [128, 520], BF16) for i in range(16)]; dVp = [Dep() for _ in range(16)]
        bias = Ring([T("nab%d" % i, [128, 2048], F32) for i in range(2)])
        EB = Ring([T("EB%d" % i, [128, 2048], BF16) for i in range(2)])
        eS = Ring([T("eS%d" % i, [128, 512], F32) for i in range(4)])
        PT = Ring([T("PTn%d" % i, [128, 512], BF16) for i in range(5)])
        yn = Ring([T("yn%d" % i, [64, 512], F32) for i in range(2)])
        ynb = Ring([T("ynb%d" % i, [64, 1, 512], BF16) for i in range(2)])
        rd = Ring([T("rdn%d" % i, [64, 8], F32) for i in range(2)])
        ys = Ring([T("ysn%d" % i, [128, 4, 512], BF16) for i in range(2)])
        ploaded = 0

        def load_q(l0):
            qT, dq = qTr.next()
            P.dma("sp", lambda e: e.dma_start(out=qT[:, :, :], in_=K.qT_d[:, l0 * 64:l0 * 64 + 512].rearrange("(c p) t -> p c t", p=128)),
                  reads=[K.dqT], writes=[dq])
            return qT, dq

        def load_bias(ti_):
            b_, db_ = bias.next()
            P.dma("sp", lambda e: e.dma_start(out=b_[:, :], in_=K.na_bias[ti_, :, :]), writes=[db_])
            return b_, db_
        own_tab = lambda lq: lq <= 4 or lq >= 60
        ntab = sum(1 for lq in range(64) if own_tab(lq))
        nb = load_bias(0); ti_ = 0
        pending = None; curT = [None]
        for lq in range(64):
            while ploaded < lq + 7:
                j = ploaded
                P.dma("sp", lambda e, j=j: e.dma_start(out=Vp[j % 16][:, :], in_=K.V_d[j * 64:j * 64 + 128, :]), reads=[K.dV], writes=[dVp[j % 16]])
                ploaded += 1
            if lq % 8 == 0:
                if lq == 0:
                    nq_ = load_q(0)
                qT, dq = nq_
            if lq % 8 == 4 and lq + 4 < 64:
                nq_ = load_q(lq + 4)
            if own_tab(lq):
                b_, db_ = nb
                P.op("act", lambda e, b_=b_: e.activation(out=b_[:, :], in_=b_[:, :], func=AF.Exp), reads=[db_], writes=[db_])
                eb_, deb_ = EB.next()
                P.op("dve", lambda e, b_=b_, eb_=eb_: e.tensor_tensor(out=eb_[:, :], in0=b_[:, :], in1=maskf[:, :], op=ALU.mult), reads=[db_, dmask], writes=[deb_])
                ti_ += 1
                if ti_ < ntab:
                    nb = load_bias(ti_)
            (pa, pb), (dpa, dpb) = pO.next()

            def qk(hp):
                ps, dps = pS.next()
                first = True
                for hh in range(2):
                    h = HORD[2 * hp + hh]
                    pb0 = 64 * (h % 2); fc = h // 2
                    for m in range(4):
                        k0 = (lq + 2 * m) * 64
                        P.op("pe", lambda e, m=m, k0=k0, hh=hh, pb0=pb0, fc=fc, ps=ps: e.matmul(
                            ps[:, hh * 256 + m * 64:hh * 256 + (m + 1) * 64], lhsT=kT[pb0:pb0 + 64, fc, k0:k0 + 128],
                            rhs=qT[pb0:pb0 + 64, fc, (lq % 8) * 64:(lq % 8 + 1) * 64], start=True, stop=True),
                            reads=[dk, dq], writes=[dps] if first else (), adds=() if first else [dps])
                        first = False
                e_, de_ = eS.next()
                P.op("act", lambda e, ps=ps, e_=e_: e.activation(out=e_[:, :], in_=ps[:, :], func=AF.Exp, scale=0.125), reads=[dps], writes=[de_])
                p_, dp_ = PT.next()
                P.op("dve", lambda e, e_=e_, p_=p_: e.tensor_tensor(out=p_[:, :], in0=e_[:, :], in1=eb_[:, hp * 512:(hp + 1) * 512], op=ALU.mult),
                     reads=[de_, deb_], writes=[dp_])
                return p_, dp_

            def pv(hp, p_, dp_):
                for hh in range(2):
                    h = HORD[2 * hp + hh]
                    po_t, dpo = (pa, dpa) if h < 4 else (pb, dpb)
                    c0 = (h % 4) * 65
                    for m in range(4):
                        kr = lq + 2 * m
                        P.op("pe", lambda e, m=m, kr=kr, h=h, hh=hh, po_t=po_t, c0=c0: e.matmul(
                            po_t[0:64, c0:c0 + 65], lhsT=p_[:, hh * 256 + m * 64:hh * 256 + (m + 1) * 64],
                            rhs=Vp[kr % 16][:, h * 65:(h + 1) * 65], start=(m == 0), stop=(m == 3)),
                            reads=[dp_, dVp[kr % 16]], writes=[dpo] if (m == 0 and h % 4 == 0) else (),
                            adds=() if (m == 0 and h % 4 == 0) else [dpo])
            curs = [qk(hp) for hp in range(4)]
            if pending is not None:
                pending(); pending = None
            for hp in range(4):
                pv(hp, *curs[hp])
            r_, dr_ = rd.next()
            P.op("dve", lambda e, r_=r_, pa=pa: e.reciprocal(r_[:, 0:4], pa[0:64, 0:260].rearrange("p (h c) -> p h c", c=65)[:, :, 64]), reads=[dpa], writes=[dr_])
            P.op("dve", lambda e, r_=r_, pb=pb: e.reciprocal(r_[:, 4:8], pb[0:64, 0:260].rearrange("p (h c) -> p h c", c=65)[:, :, 64]), reads=[dpb], adds=[dr_])
            y_, dy_ = yn.next()
            for h in range(8):
                po_t, dpo = (pa, dpa) if h < 4 else (pb, dpb)
                c0 = (h % 4) * 65
                P.op("dve", lambda e, h=h, po_t=po_t, c0=c0, y_=y_, r_=r_: e.tensor_scalar_mul(y_[:, h * 64:(h + 1) * 64], po_t[0:64, c0:c0 + 64], r_[:, h:h + 1]),
                     reads=[dpo, dr_], writes=[dy_] if h == 0 else (), adds=[dy_] if h else ())
            yb_, dyb_ = ynb.next()
            norm_tiles(K, [y_[:, :]], dy_, yb_, dyb_, gg, dgg, 512, np_=64)

            def row_end(lq=lq, yb_=yb_, dyb_=dyb_):
                sub = lq % 8
                if sub == 0:
                    pTa, dpTa = K.pT.next()
                    pTb, dpTb = K.pT.next()
                    curT[0] = (pTa, dpTa, pTb, dpTb)
                pTa, dpTa, pTb, dpTb = curT[0]
                for fc in range(4):
                    pt, dpt = (pTa, dpTa) if fc < 2 else (pTb, dpTb)
                    o = (fc % 2) * 512 + sub * 64
                    P.op("pe", lambda e, fc=fc, pt=pt, o=o: e.transpose(pt[:, o:o + 64], yb_[:, 0, fc * 128:(fc + 1) * 128], K.ident[0:64, 0:64]),
                         reads=[dyb_, K.dconst], writes=[dpt] if (sub == 0 and fc % 2 == 0) else (),
                         adds=() if (sub == 0 and fc % 2 == 0) else [dpt])
                if sub == 7:
                    s_, ds_ = ys.next()
                    copy_op(K, "act", s_[:, 0:2, :], pTa[:, :].rearrange("p (c w) -> p c w", w=512), reads=[dpTa], writes=[ds_])
                    copy_op(K, "dve", s_[:, 2:4, :], pTb[:, :].rearrange("p (c w) -> p c w", w=512), reads=[dpTb], adds=[ds_])
                    t0 = (lq // 8) * 512
                    P.dma("pool", lambda e, t0=t0, s_=s_: e.dma_start(out=K.yT_d[0:512, t0:t0 + 512].rearrange("(c p) t -> p c t", p=128), in_=s_[:, :, :]),
                          reads=[ds_], adds=[K.dyT])
            pending = row_end
        pending()


def phase_FT(K):
    nc, P = K.nc, K.P
    with ExitStack() as st:
        T = lambda name, shape, dt: st.enter_context(nc.sbuf_tensor("sb_" + name, shape, dt))
        uT = T("uT", [128, 4, SEQ], BF16); du = Dep()
        for g in range(4):
            for hh in range(2):
                P.dma("sp", lambda e, g=g, hh=hh: e.dma_start(out=uT[:, g, hh * 4096:(hh + 1) * 4096], in_=K.uT_d[g * 128:(g + 1) * 128, hh * 4096:(hh + 1) * 4096]),
                      reads=[K.duT], adds=[du])
        cs4 = T("cs4", [128, 512], BF16); E = T("Etw", [128, 64, 2, 128], BF16); dc = Dep()
        P.dma("sp", lambda e: e.dma_start(out=cs4[:, :], in_=K.cs4[:, :]), adds=[dc])
        P.dma("sp", lambda e: e.dma_start(out=E[:, :, :, :], in_=K.etw[:, :, :, :]), adds=[dc])
        Z = Ring([T("Z%d" % i, [128, 512], BF16) for i in range(3)])
        Bs = Ring([T("Bs%d" % i, [128, 2, 128], BF16) for i in range(3)])
        def st1(s1, g):
            pf, dpf = K.pF.next()
            lhs = uT[:, g, :].rearrange("p (s2 s1) -> p s1 s2", s1=64)[:, s1, :]
            P.op("pe", lambda e, lhs=lhs, pf=pf: e.matmul(pf[:, :], lhsT=lhs, rhs=cs4[:, :], start=True, stop=True), reads=[du, dc], writes=[dpf])
            z_, dz_ = Z.next()
            copy_op(K, _alt(K), z_[:, :], pf[:, :], reads=[dpf], writes=[dz_])
            return z_, dz_

        def st2(s1, g, z_, dz_):
            pf2, dpf2 = K.pF.next()
            P.op("pe", lambda e, pf2=pf2: e.matmul(pf2[:, 0:256], lhsT=E[:, s1, 0, :], rhs=z_[:, 0:256], start=True, stop=False), reads=[dz_, dc], writes=[dpf2])
            P.op("pe", lambda e, pf2=pf2: e.matmul(pf2[:, 0:256], lhsT=E[:, s1, 1, :], rhs=z_[:, 256:512], start=False, stop=True), reads=[dz_, dc], adds=[dpf2])
            b_, db_ = Bs.next()
            copy_op(K, _alt(K), b_[:, :, :], pf2[:, 0:256].rearrange("p (r c) -> p r c", c=128), reads=[dpf2], writes=[db_])
            P.dma("pool", lambda e, g=g, b_=b_: e.dma_start(out=K.B_d[g, :, s1, :, :].rearrange("r k c -> k r c"), in_=b_[:, :, :]),
                  reads=[db_], adds=[K.dB])
        units = [(s1, g) for s1 in range(64) for g in range(4)]
        zc = st1(*units[0])
        for i, u in enumerate(units):
            zn = st1(*units[i + 1]) if i + 1 < len(units) else None
            st2(*u, *zc)
            zc = zn
        P.barrier()
    with ExitStack() as st:
        T = lambda name, shape, dt: st.enter_context(nc.sbuf_tensor("sb_" + name, shape, dt))
        K.yb = Ring([T("ybf%d" % i, [128, 4, 512], BF16) for i in range(2)])
        K.ystage = Ring([T("ystf%d" % i, [128, 4, 512], BF16) for i in range(2)])
        Dm = T("Dm", [128, 32], BF16); dDm = Dep()
        P.dma("sp", lambda e: e.dma_start(out=Dm[:, :], in_=K.dmat[:, :]), writes=[dDm])
        gg = T("ggf", [128, 512], F32); dgg = Dep()
        P.dma("sp", lambda e: e.dma_start(out=gg[:, :], in_=K.ggrp[:, 512:1024]), writes=[dgg])
        Bt = Ring([T("Bt%d" % i, [128, 128, 128], BF16) for i in range(2)])
        yft = T("yft", [128, 32, 512], F32); dyft = Dep()
        def load_bt(g):
            bt_, dbt_ = Bt.next()
            for hh in range(2):
                P.dma("sp", lambda e, hh=hh: e.dma_start(out=bt_[hh * 64:(hh + 1) * 64, :, :], in_=K.B_d[g, hh, :, :, :]), reads=[K.dB],
                      writes=[dbt_] if hh == 0 else (), adds=[dbt_] if hh else ())
            return bt_, dbt_
        nbt = load_bt(0)
        for g in range(4):
            bt_, dbt_ = nbt
            if g < 3:
                nbt = load_bt(g + 1)
            for cb in range(8):
                pf, dpf = K.pF.next()
                for cc in range(16):
                    c = cb * 16 + cc
                    P.op("pe", lambda e, c=c, cc=cc, pf=pf: e.matmul(pf[:, cc * 32:(cc + 1) * 32], lhsT=bt_[:, :, c], rhs=Dm[:, :], start=True, stop=True),
                         reads=[dbt_, dDm], writes=[dpf] if cc == 0 else (), adds=() if cc == 0 else [dpf])
                c0 = g * 128 + cb * 16
                copy_op(K, _alt(K), yft[:, :, c0:c0 + 16], pf[:, :].rearrange("p (c k) -> p k c", k=32), reads=[dpf],
                        writes=[dyft] if (g == 0 and cb == 0) else (), adds=() if (g == 0 and cb == 0) else [dyft])
        for k4 in range(8):
            emit_y(K, [yft[:, k4 * 4 + t, :] for t in range(4)], dyft, gg, dgg, 512, k4 * 512)


def phase_OUT(K):
    nc, P = K.nc, K.P
    with ExitStack() as st:
        T = lambda name, shape, dt: st.enter_context(nc.sbuf_tensor("sb_" + name, shape, dt))
        wo = T("wo", [128, 12, 1024], BF16); dwo = Dep()
        for fc in range(12):
            P.dma("pool", lambda e, fc=fc: e.dma_start(out=wo[:, fc, :], in_=K.w_out[fc * 128:(fc + 1) * 128, :]), adds=[dwo])
        rw = T("rw", [128, 8, 32], BF16); drw = Dep()
        P.dma("pool", lambda e: e.dma_start(out=rw[:, :, :], in_=K.router_w[:, :].rearrange("(c p) e -> p c e", p=128)), writes=[drw])
        cst = T("ocst", [128, 1024 + 32 + 32], F32); dcst = Dep()
        gf = cst[:, 0:1024]; rb = cst[:, 1024:1056]; iota = cst[:, 1056:1088]
        P.dma("sp", lambda e: e.dma_start(out=gf, in_=K.gffn[:, :]), adds=[dcst])
        P.dma("sp", lambda e: e.dma_start(out=rb, in_=K.router_b[:, :]), adds=[dcst])
        P.dma("sp", lambda e: e.dma_start(out=iota, in_=K.iota[:, :]), adds=[dcst])
        LO = T("LO", [128, 2, 128], BF16); dLO = Dep()
        P.dma("sp", lambda e: e.dma_start(out=LO[:, :, :], in_=K.lo[:, :, :]), writes=[dLO])
        base = T("base", [128, 32], F32); dbase = Dep()
        P.op("pool", lambda e: e.memset(base[:, :], 0.0), writes=[dbase])
        yT = Ring([T("yTo%d" % i, [128, 12, 512], BF16) for i in range(2)])
        xr = Ring([T("xro%d" % i, [128, 4, 1024], F32) for i in range(2)])
        x1 = Ring([T("x1o%d" % i, [128, 1024], F32) for i in range(2)])
        h2all = T("h2all", [128, 32, 1024], BF16); dh2 = [Dep() for _ in range(32)]
        ekall = T("ekall", [128, 128], F32); slall = T("slall", [128, 128], F32); dek = Dep()
        rofs = T("rofs", [128, 8], F32); drofs = Dep()
        P.dma("sp", lambda e: e.dma_start(out=rofs[:, :], in_=K.rowoff[:, :]), writes=[drofs])
        h2T = Ring([T("h2T%d" % i, [128, 8, 128], BF16) for i in range(2)])
        sm = Ring([T("sm%d" % i, [128, 416], F32) for i in range(2)])
        Mb = Ring([T("Mb%d" % i, [128, 32], BF16) for i in range(2)])

        def load(blk):
            y_, dy_ = yT.next()
            P.dma("sp", lambda e: e.dma_start(out=y_[:, :, :], in_=K.yT_d[:, blk * 512:(blk + 1) * 512].rearrange("(c p) t -> p c t", p=128)),
                  reads=[K.dyT], writes=[dy_])
            x_, dx_ = xr.next()
            P.dma("sp", lambda e: e.dma_start(out=x_[:, :, :], in_=K.x_core[blk * 512:(blk + 1) * 512, :].rearrange("(t p) d -> p t d", p=128)),
                  writes=[dx_])
            return y_, dy_, x_, dx_
        tiles = [(blk, t) for blk in range(8) for t in range(4)]
        loaded = {}

        def get_blk(blk):
            if blk not in loaded:
                loaded[blk] = load(blk)
            return loaded[blk]
        get_blk(0)

        def stageA(blk, t):
            ti = blk * 4 + t
            y_, dy_, x_, dx_ = get_blk(blk)
            if t == 0 and blk < 7:
                get_blk(blk + 1)
            x1_, dx1_ = x1.next()
            for dh in range(2):
                pf, dpf = K.pF.next()
                for fc in range(12):
                    P.op("pe", lambda e, fc=fc, dh=dh, pf=pf: e.matmul(pf[:, :], lhsT=y_[:, fc, t * 128:(t + 1) * 128], rhs=wo[:, fc, dh * 512:(dh + 1) * 512],
                                                                       start=(fc == 0), stop=(fc == 11)),
                         reads=[dy_, dwo], writes=[dpf] if fc == 0 else (), adds=() if fc == 0 else [dpf])
                P.op("dve", lambda e, dh=dh, pf=pf: e.tensor_tensor(out=x1_[:, dh * 512:(dh + 1) * 512], in0=pf[:, :], in1=x_[:, t, dh * 512:(dh + 1) * 512], op=ALU.add),
                     reads=[dpf, dx_], writes=[dx1_] if dh == 0 else (), adds=[dx1_] if dh else ())
            P.dma("pool", lambda e: e.dma_start(out=K.x1_d[ti * 128:(ti + 1) * 128, :], in_=x1_[:, :]), reads=[dx1_], adds=[K.dx1])
            stat, dstat = K.stat.next()
            P.op("act", lambda e: e.activation(out=K.junk[:, 0:1024], in_=x1_[:, :], func=AF.Square, accum_out=stat[:, 0:1]),
                 reads=[dx1_], writes=[dstat, K.djunk])
            return ti, x1_, dx1_, stat, dstat

        def stageB(ti, x1_, dx1_, stat, dstat):
            dhb_ = dh2[ti]
            hb3 = h2all[:, ti:ti + 1, :]
            P.op("dve", lambda e: e.tensor_scalar(out=stat[:, 1:2], in0=stat[:, 0:1], scalar1=1.0 / 1024, scalar2=1e-6, op0=ALU.mult, op1=ALU.add),
                 reads=[dstat], writes=[dstat])
            P.op("act", lambda e: e.sqrt(stat[:, 1:2], stat[:, 1:2]), reads=[dstat], writes=[dstat])
            P.op("dve", lambda e: e.reciprocal(stat[:, 1:2], stat[:, 1:2]), reads=[dstat], writes=[dstat])
            P.op("dve", lambda e: e.scalar_tensor_tensor(out=hb3[:, 0, :], in0=x1_[:, :], scalar=stat[:, 1:2], in1=gf, op0=ALU.mult, op1=ALU.mult),
                 reads=[dx1_, dstat, dcst], writes=[dhb_])
            hT_, dhT_ = h2T.next()
            transpose_block(K, hb3, dhb_, 1, 8, hT_, dhT_)
            pl, dpl = K.pF.next()
            for kc in range(8):
                P.op("pe", lambda e, kc=kc, pl=pl: e.matmul(pl[:, 0:32], lhsT=hT_[:, kc, :], rhs=rw[:, kc, :], start=(kc == 0), stop=(kc == 7)),
                     reads=[dhT_, drw], writes=[dpl] if kc == 0 else (), adds=() if kc == 0 else [dpl])
            s_, ds_ = sm.next()
            lg = s_[:, 0:32]
            P.op("dve", lambda e, pl=pl: e.tensor_tensor(out=lg, in0=pl[:, 0:32], in1=rb, op=ALU.add), reads=[dpl, dcst], writes=[ds_])

            def route(ti=ti, s_=s_, ds_=ds_):
                lg = s_[:, 0:32]; t8 = s_[:, 32:40]; slot = s_[:, 40:72]; junk = s_[:, 72:104]
                sl = s_[:, 104:108]; ek = s_[:, 108:112]
                nmx = s_[:, 120:121]; gs = s_[:, 121:122]; e4 = s_[:, 124:128]
                P.op("dve", lambda e: e.max(out=t8, in_=lg), reads=[ds_], writes=[ds_])
                m_, dm_ = Mb.next()
                P.op("dve", lambda e: e.tensor_scalar(out=m_[:, :], in0=lg, scalar1=s_[:, 35:36], scalar2=1.0, op0=ALU.is_ge, op1=ALU.mult), reads=[ds_], writes=[dm_])
                pp, dpp = K.pF.next()
                P.op("pe", lambda e, pp=pp: e.matmul(pp[:, 0:32], lhsT=LO[:, 0, :], rhs=m_[:, :], start=True, stop=True), reads=[dLO, dm_], writes=[dpp])
                P.op("pe", lambda e, pp=pp: e.matmul(pp[:, 32:64], lhsT=LO[:, 1, :], rhs=m_[:, :], start=True, stop=True), reads=[dLO, dm_], adds=[dpp])
                P.op("dve", lambda e, pp=pp: e.tensor_tensor(out=slot, in0=pp[:, 0:32], in1=base[:, :], op=ALU.add), reads=[dpp, dbase, ds_], writes=[ds_])
                P.op("dve", lambda e, pp=pp: e.tensor_tensor(out=base[:, :], in0=pp[:, 32:64], in1=base[:, :], op=ALU.add), reads=[dpp, dbase], writes=[dbase])
                dse = Dep()
                for k in range(4):
                    P.op("dve", lambda e, k=k: e.scalar_tensor_tensor(out=s_[:, 160 + 64 * k:192 + 64 * k], in0=lg, scalar=s_[:, 32 + k:33 + k], in1=slot,
                                                                      op0=ALU.is_equal, op1=ALU.mult, accum_out=sl[:, k:k + 1]), reads=[ds_], adds=[dse])
                    P.op("dve", lambda e, k=k: e.scalar_tensor_tensor(out=s_[:, 192 + 64 * k:224 + 64 * k], in0=lg, scalar=s_[:, 32 + k:33 + k], in1=iota,
                                                                      op0=ALU.is_equal, op1=ALU.mult, accum_out=ek[:, k:k + 1]), reads=[ds_, dcst], adds=[dse])
                P.op("dve", lambda e, ti=ti: e.tensor_copy(ekall[:, ti * 4:ti * 4 + 4], ek), reads=[dse], adds=[dek])
                P.op("dve", lambda e, ti=ti: e.tensor_copy(slall[:, ti * 4:ti * 4 + 4], sl), reads=[dse], adds=[dek])
                P.op("dve", lambda e: e.tensor_scalar_mul(nmx, s_[:, 32:33], -1.0), reads=[ds_], writes=[ds_])
                P.op("act", lambda e: e.activation(out=e4, in_=s_[:, 32:36], func=AF.Exp, bias=nmx, scale=1.0, accum_out=gs), reads=[ds_], writes=[ds_])
                P.op("dve", lambda e: e.reciprocal(gs, gs), reads=[ds_], writes=[ds_])
                P.op("dve", lambda e, ti=ti: e.tensor_scalar_mul(K.gates_all[:, ti, :], e4, gs), reads=[ds_], adds=[K.dgates])
            return route
        pend_route = None
        curA = stageA(*tiles[0])
        for i in range(len(tiles)):
            nxtA = stageA(*tiles[i + 1]) if i + 1 < len(tiles) else None
            r = stageB(*curA)
            if pend_route is not None:
                pend_route()
            pend_route = r
            curA = nxtA
        pend_route()
        rt = T("rt", [128, 512], F32); drt = Dep()
        nblk = rt[:, 0:32]; c0 = rt[:, 32:64]; c1 = rt[:, 64:96]; pstart = rt[:, 96:128]; one32 = rt[:, 128:160]; j32 = rt[:, 160:192]
        bexp = rt[:, 192:256]; bex1024 = rt[:, 256:320]; bex128 = rt[:, 320:384]; tmp64 = rt[:, 384:448]
        P.op("dve", lambda e: e.memset(rt[:, :], 0.0), writes=[drt])
        P.op("dve", lambda e: e.memset(one32, 1.0), writes=[drt])
        for j in range(8):
            P.op("dve", lambda e, j=j: e.scalar_tensor_tensor(out=nblk, in0=base[:, :], scalar=float(BLK * j), in1=nblk, op0=ALU.is_gt, op1=ALU.add),
                 reads=[dbase, drt], writes=[drt])
        P.op("dve", lambda e: e.tensor_copy(c0, nblk), reads=[drt], writes=[drt])
        src, dst = c0, c1
        for sft in (1, 2, 4, 8, 16):
            P.op("dve", lambda e, src=src, dst=dst, sft=sft: e.tensor_copy(dst[:, 0:sft], src[:, 0:sft]), reads=[drt], writes=[drt])
            P.op("dve", lambda e, src=src, dst=dst, sft=sft: e.tensor_tensor(out=dst[:, sft:32], in0=src[:, sft:32], in1=src[:, 0:32 - sft], op=ALU.add),
                 reads=[drt], writes=[drt])
            src, dst = dst, src
        cb = src
        P.op("dve", lambda e: e.tensor_tensor(out=pstart, in0=cb, in1=nblk, op=ALU.subtract), reads=[drt], writes=[drt])
        P.op("dve", lambda e: e.tensor_scalar_mul(pstart, pstart, float(BLK)), reads=[drt], writes=[drt])
        dstf = T("dstf", [128, 128], F32); ddf = Dep()
        jbig = T("jbig", [128, 128 * 32], F32)
        for col in range(128):
            P.op("dve", lambda e, col=col: e.scalar_tensor_tensor(out=jbig[:, col * 32:(col + 1) * 32], in0=iota, scalar=ekall[:, col:col + 1], in1=pstart,
                                                                  op0=ALU.is_equal, op1=ALU.mult, accum_out=dstf[:, col:col + 1]),
                 reads=[drt, dek, dcst], adds=[ddf])
        P.op("dve", lambda e: e.tensor_tensor(out=dstf[:, :], in0=dstf[:, :], in1=slall[:, :], op=ALU.add), reads=[ddf, dek], writes=[ddf, drt])
        P.op("dve", lambda e: e.tensor_copy(K.dst_all[:, :], dstf[:, :]), reads=[drt], adds=[K.ddst])
        for ti in range(32):
            for k in range(4):
                P.dma("pool", lambda e, k=k, ti=ti: e.indirect_dma_start(
                    out=K.Xs_d[:, :], out_offset=bass.IndirectOffsetOnAxis(ap=K.dst_all[:, ti * 4 + k:ti * 4 + k + 1], axis=0),
                    in_=h2all[:, ti, :], in_offset=None), reads=[dh2[ti], K.ddst], adds=[K.dXs])
        for b in range(NBLK):
            P.op("dve", lambda e, b=b: e.scalar_tensor_tensor(out=j32, in0=cb, scalar=float(b), in1=one32, op0=ALU.is_le, op1=ALU.mult,
                                                              accum_out=bexp[:, b:b + 1]), reads=[drt], writes=[drt])
        P.op("dve", lambda e: e.tensor_scalar_mul(bex1024, bexp, 1024.0), reads=[drt], writes=[drt])
        P.op("dve", lambda e: e.tensor_scalar_mul(bex128, bexp, 128.0), reads=[drt], writes=[drt])
        widxf = T("widxf", [128, NBLK * 8], F32)
        for b in range(NBLK):
            P.op("dve", lambda e, b=b: e.tensor_scalar_add(widxf[:, b * 8:(b + 1) * 8], rofs[:, :], bex1024[:, b:b + 1]), reads=[drt, drofs], writes=[drt])
        P.op("dve", lambda e: e.tensor_copy(K.widx[:, :], widxf[:, :]), reads=[drt], adds=[K.dwidx])
        P.op("dve", lambda e: e.tensor_scalar_add(tmp64, bex128, rofs[:, 0:1]), reads=[drt, drofs], writes=[drt])
        P.op("dve", lambda e: e.tensor_copy(K.bgidx[:, :], tmp64[:, 0:NBLK]), reads=[drt], adds=[K.dwidx])
        P.op("dve", lambda e: e.tensor_copy(K.bdidx[:, :], bexp[:, 0:NBLK]), reads=[drt], adds=[K.dwidx])


def moe_T(K, R, x_, dx_):
    P = K.P
    xt_, dxt_ = R["XT"].next()
    for s in range(4):
        pT, dpT = K.pT.next()
        for kc in range(8):
            P.op("pe", lambda e, kc=kc, s=s, pT=pT: e.transpose(pT[:, kc * 128:(kc + 1) * 128], x_[:, s, kc * 128:(kc + 1) * 128], K.ident[:, :]),
                 reads=[dx_, K.dconst], writes=[dpT] if kc == 0 else (), adds=() if kc == 0 else [dpT])
        copy_op(K, _alt(K), xt_[:, :, s * 128:(s + 1) * 128], pT[:, :].rearrange("p (c w) -> p c w", w=128), reads=[dpT],
                writes=[dxt_] if s == 0 else (), adds=[dxt_] if s else ())
    return xt_, dxt_


def moe_gu(K, R, g_, dg_, bgt, dbgt, xt_, dxt_):
    P = K.P
    a_, da_ = R["aT"].next()
    for fc in range(8):
        pg, dpg = K.pF.next()
        pl, dpl = K.pF.next()
        for kc in range(8):
            P.op("pe", lambda e, kc=kc, pg=pg: e.matmul(pg[:, :], lhsT=g_[:, kc, fc * 128:(fc + 1) * 128], rhs=xt_[:, kc, :], start=(kc == 0), stop=(kc == 7)),
                 reads=[dg_, dxt_], writes=[dpg] if kc == 0 else (), adds=() if kc == 0 else [dpg])
        for kc in range(8):
            P.op("pe", lambda e, kc=kc, pl=pl: e.matmul(pl[:, :], lhsT=g_[:, kc, 1024 + fc * 128:1024 + (fc + 1) * 128], rhs=xt_[:, kc, :], start=(kc == 0), stop=(kc == 7)),
                 reads=[dg_, dxt_], writes=[dpl] if kc == 0 else (), adds=() if kc == 0 else [dpl])
        A, dA = R["ta"].next(); S_, dS = R["ts"].next(); L, dL = R["tl"].next()
        P.op("act", lambda e, pg=pg, A=A: e.activation(out=A[:, :], in_=pg[:, :], func=AF.Identity, bias=bgt[:, fc:fc + 1], scale=1.0),
             reads=[dpg, dbgt], writes=[dA])
        P.op("act", lambda e, pl=pl, L=L: e.activation(out=L[:, :], in_=pl[:, :], func=AF.Identity, bias=bgt[:, 8 + fc:9 + fc], scale=1.0),
             reads=[dpl, dbgt], writes=[dL])
        P.op("dve", lambda e, A=A: e.tensor_scalar_min(A[:, :], A[:, :], 7.0), reads=[dA], writes=[dA])
        P.op("act", lambda e, A=A, S_=S_: e.activation(out=S_[:, :], in_=A[:, :], func=AF.Sigmoid, scale=1.702), reads=[dA], writes=[dS])
        P.op("dve", lambda e, L=L: e.tensor_scalar(out=L[:, :], in0=L[:, :], scalar1=7.0, scalar2=-7.0, op0=ALU.min, op1=ALU.max), reads=[dL], writes=[dL])
        P.op("dve", lambda e, A=A, S_=S_: e.tensor_tensor(out=S_[:, :], in0=A[:, :], in1=S_[:, :], op=ALU.mult), reads=[dA, dS], writes=[dS])
        P.op("dve", lambda e, S_=S_, L=L: e.scalar_tensor_tensor(out=a_[:, fc, :], in0=L[:, :], scalar=1.0, in1=S_[:, :], op0=ALU.add, op1=ALU.mult),
             reads=[dS, dL], writes=[da_] if fc == 0 else (), adds=() if fc == 0 else [da_])
    return a_, da_


def moe_down(K, R, d_, dd_, b_, db_, a_, da_, row0):
    P = K.P
    for s in range(4):
        y_, dy_ = R["yo"].next()
        for dh in range(2):
            pd, dpd = K.pF.next()
            for fc in range(8):
                P.op("pe", lambda e, fc=fc, pd=pd: e.matmul(pd[:, :], lhsT=a_[:, fc, s * 128:(s + 1) * 128], rhs=d_[:, fc, dh * 512:(dh + 1) * 512],
                                                            start=(fc == 0), stop=(fc == 7)),
                     reads=[da_, dd_], writes=[dpd] if fc == 0 else (), adds=() if fc == 0 else [dpd])
            P.op("dve", lambda e, pd=pd, dh=dh: e.tensor_tensor(out=y_[:, dh * 512:(dh + 1) * 512], in0=pd[:, :], in1=b_[:, dh * 512:(dh + 1) * 512], op=ALU.add),
                 reads=[dpd, db_], writes=[dy_] if dh == 0 else (), adds=[dy_] if dh else ())
        r0 = row0 + s * 128
        P.dma("sp", lambda e, r0=r0, y_=y_: e.dma_start(out=K.Y_d[r0:r0 + 128, :], in_=y_[:, :]), reads=[dy_], adds=[K.dY])


def phase_MOE(K):
    nc, P = K.nc, K.P
    IO = bass.IndirectOffsetOnAxis
    with ExitStack() as st:
        T = lambda name, shape, dt: st.enter_context(nc.sbuf_tensor("sb_" + name, shape, dt))
        wg = Ring([T("wg%d" % i, [128, 8, 2048], BF16) for i in range(2)])
        wd = Ring([T("wd%d" % i, [128, 8, 1024], BF16) for i in range(2)])
        bdb = Ring([T("bdb%d" % i, [128, 1024], F32) for i in range(2)])
        bgr = Ring([T("bgr%d" % i, [128, 16], F32) for i in range(2)])
        Xe = Ring([T("Xe%d" % i, [128, 4, 1024], BF16) for i in range(2)])
        R = dict(XT=Ring([T("XT%d" % i, [128, 8, 512], BF16) for i in range(2)]),
                 aT=Ring([T("aT%d" % i, [128, 8, 512], BF16) for i in range(2)]),
                 ta=Ring([T("ta%d" % i, [128, 512], F32) for i in range(2)]),
                 ts=Ring([T("ts%d" % i, [128, 512], F32) for i in range(2)]),
                 tl=Ring([T("tl%d" % i, [128, 512], F32) for i in range(2)]),
                 yo=Ring([T("yo%d" % i, [128, 1024], F32) for i in range(2)]))

        def load_w(b):
            g_, dg_ = wg.next()
            for kc in range(8):
                P.dma("pool", lambda e, kc=kc: e.indirect_dma_start(out=g_[:, kc, :], out_offset=None, in_=K.w_gu[:, :],
                                                                    in_offset=IO(ap=K.widx[:, b * 8 + kc:b * 8 + kc + 1], axis=0), bounds_check=K.reg_w, oob_is_err=False),
                      reads=[K.dwidx], writes=[dg_] if kc == 0 else (), adds=[dg_] if kc else ())
            d_, dd_ = wd.next()
            for kc in range(8):
                P.dma("pool", lambda e, kc=kc: e.indirect_dma_start(out=d_[:, kc, :], out_offset=None, in_=K.w_down[:, :],
                                                                    in_offset=IO(ap=K.widx[:, b * 8 + kc:b * 8 + kc + 1], axis=0), bounds_check=K.reg_w, oob_is_err=False),
                      reads=[K.dwidx], writes=[dd_] if kc == 0 else (), adds=[dd_] if kc else ())
            b_, db_ = bdb.next()
            P.dma("pool", lambda e: e.indirect_dma_start(out=b_[:, :], out_offset=None, in_=K.b_down[:, :], in_offset=IO(ap=K.bdidx[:, b:b + 1], axis=0), bounds_check=K.reg_bd, oob_is_err=False),
                  reads=[K.dwidx], writes=[db_])
            t_, dt_ = bgr.next()
            P.dma("pool", lambda e: e.indirect_dma_start(out=t_[:, :], out_offset=None, in_=K.b_gu[:, :], in_offset=IO(ap=K.bgidx[:, b:b + 1], axis=0), bounds_check=K.reg_bg, oob_is_err=False),
                  reads=[K.dwidx], writes=[dt_])
            return g_, dg_, d_, dd_, b_, db_, t_, dt_

        def load_x(bi):
            x_, dx_ = Xe.next()
            P.dma("sp", lambda e: e.dma_start(out=x_[:, :, :], in_=K.Xs_d[bi * 512:(bi + 1) * 512, :].rearrange("(t p) d -> p t d", p=128)),
                  reads=[K.dXs], writes=[dx_])
            return x_, dx_
        W = {0: load_w(0)}
        X = {0: load_x(0)}
        W[1] = load_w(1); X[1] = load_x(1)
        xt = {0: moe_T(K, R, *X[0])}
        aT = {0: moe_gu(K, R, W[0][0], W[0][1], W[0][6], W[0][7], *xt[0])}
        for b in range(NBLK):
            if b + 1 < NBLK:
                xt[b + 1] = moe_T(K, R, *X[b + 1])
            g_, dg_, d_, dd_, b_, db_, t_, dt_ = W[b]
            moe_down(K, R, d_, dd_, b_, db_, *aT[b], b * 512)
            if b + 2 < NBLK:
                W[b + 2] = load_w(b + 2); X[b + 2] = load_x(b + 2)
            if b + 1 < NBLK:
                w1 = W[b + 1]
                aT[b + 1] = moe_gu(K, R, w1[0], w1[1], w1[6], w1[7], *xt[b + 1])


def phase_FINAL(K):
    nc, P = K.nc, K.P
    with ExitStack() as st:
        T = lambda name, shape, dt: st.enter_context(nc.sbuf_tensor("sb_" + name, shape, dt))
        gfin = T("gfin", [128, 1024], F32); dgf = Dep()
        P.dma("sp", lambda e: e.dma_start(out=gfin[:, :], in_=K.gfin[:, :]), writes=[dgf])
        Yk = Ring([T("Yk%d" % i, [128, 1024], F32) for i in range(8)])
        x1 = Ring([T("x1f%d" % i, [128, 1024], F32) for i in range(2)])
        acc = Ring([T("acc%d" % i, [128, 1024], F32) for i in range(2)])
        ob = Ring([T("ob%d" % i, [128, 1, 1024], F32) for i in range(2)])
        stat = Ring([T("stf%d" % i, [128, 2], F32) for i in range(2)])
        for ti in range(32):
            x_, dx_ = x1.next()
            P.dma("sp", lambda e: e.dma_start(out=x_[:, :], in_=K.x1_d[ti * 128:(ti + 1) * 128, :]), reads=[K.dx1], writes=[dx_])
            ys = []
            for k in range(4):
                y_, dy_ = Yk.next()
                P.dma("pool", lambda e, k=k, y_=y_: e.indirect_dma_start(
                    out=y_[:, :], out_offset=None, in_=K.Y_d[:, :], in_offset=bass.IndirectOffsetOnAxis(ap=K.dst_all[:, ti * 4 + k:ti * 4 + k + 1], axis=0)),
                    reads=[K.dY, K.ddst], writes=[dy_])
                ys.append((y_, dy_))
            a_, da_ = acc.next()
            prev, dprev = x_, dx_
            for k in range(4):
                y_, dy_ = ys[k]
                eng = "dve"
                P.op(eng, lambda e, k=k, y_=y_, prev=prev: e.scalar_tensor_tensor(out=a_[:, :], in0=y_[:, :], scalar=K.gates_all[:, ti, k:k + 1], in1=prev[:, :],
                                                                                 op0=ALU.mult, op1=ALU.add),
                     reads=[dy_, dprev, K.dgates], writes=[da_])
                prev, dprev = a_, da_
            s_, ds_ = stat.next()
            P.op("act", lambda e: e.activation(out=K.junk[:, 0:1024], in_=a_[:, :], func=AF.Square, accum_out=s_[:, 0:1]), reads=[da_], writes=[ds_, K.djunk])
            rstd_ops(K, s_, ds_, 1, 1024)
            o_, do_ = ob.next()
            P.op("dve", lambda e: e.scalar_tensor_tensor(out=o_[:, 0, :], in0=a_[:, :], scalar=s_[:, 1:2], in1=gfin[:, :], op0=ALU.mult, op1=ALU.mult),
                 reads=[da_, ds_, dgf], writes=[do_])
            P.dma("sp", lambda e: e.dma_start(out=K.out[ti * 128:(ti + 1) * 128, :], in_=o_[:, 0, :]), reads=[do_], adds=[K.dout])


def build(stop_after=None, lite=False):
    nc = bass.Bass("TRN2", target_bir_lowering=False)
    K = Ctx(); K.nc = nc; K.alt = 0
    ein = lambda name, shape, dt=F32: nc.dram_tensor(name, shape, dt, kind="ExternalInput").ap()
    scr = lambda name, shape, dt: nc.dram_tensor(name, shape, dt, kind="ExternalOutput" if DEBUG else "Internal").ap()
    K.x_core = ein("x_core", [SEQ, D]); K.x_kv = ein("x_kv", [KVTOK, D]); K.mem = ein("mem", [256, D])
    K.gmix = ein("gmix", [128, D]); K.gmem = ein("gmem", [128, D]); K.ggrp = ein("ggrp", [128, 1536])
    K.gffn = ein("gffn", [128, D]); K.gfin = ein("gfin", [128, D]); K.router_b = ein("router_b", [128, NEXP])
    K.w_in = ein("w_in", [D, 2560]); K.w_kv = ein("w_kv", [D, 1024]); K.w_out = ein("w_out", [1536, D])
    ne = 1 if lite else NEXP
    K.router_w = ein("router_w", [D, NEXP]); K.w_gu = ein("w_gu", [ne * D, 2048]); K.b_gu = ein("b_gu", [NEXP * 128, 16])
    K.w_down = ein("w_down", [ne * D, D]); K.b_down = ein("b_down", [NEXP, D]); K.rowoff = ein("rowoff", [128, 8])
    K.na_bias = ein("na_bias", [9, 128, 2048]); K.na_mask = ein("na_mask", [128, 2048], BF16)
    K.cs4 = ein("cs4", [128, 512], BF16); K.etw = ein("etw", [128, 64, 2, 128], BF16); K.dmat = ein("dmat", [128, 32], BF16)
    K.identd = ein("ident", [128, 128], BF16); K.lo = ein("lo", [128, 2, 128], BF16); K.iota = ein("iota", [128, NEXP])
    K.out = nc.dram_tensor("out", [TOK, D], F32, kind="ExternalOutput").ap()
    K.qT_d = scr("qT_d", [512, TOK], BF16); K.kT_d = scr("kT_d", [512, KVTOK], BF16); K.V_d = scr("V_d", [KVTOK, 520], BF16)
    K.uT_d = scr("uT_d", [512, SEQ], BF16); K.qmT_d = scr("qmT_d", [512, TOK], BF16); K.yT_d = scr("yT_d", [1536, TOK], BF16)
    K.B_d = scr("B_d", [4, 2, 64, 128, 128], BF16); K.x1_d = scr("x1_d", [TOK, D], F32)
    K.Xs_d = scr("Xs_d", [NBLK * BLK, D], BF16); K.Y_d = scr("Y_d", [NBLK * BLK, D], F32)
    for n in ("dqT", "dkT", "dV", "duT", "dqmT", "dyT", "dB", "dx1", "dXs", "dY", "dout", "ddst", "dgates", "dconst", "djunk", "dwidx"):
        setattr(K, n, Dep())
    with ExitStack() as st:
        P = Prog(nc, st); K.P = P
        T = lambda name, shape, dt: st.enter_context(nc.sbuf_tensor("sb_" + name, shape, dt))
        K.ident = T("ident_sb", [128, 128], BF16)
        P.dma("sp", lambda e: e.dma_start(out=K.ident[:, :], in_=K.identd[:, :]), writes=[K.dconst])
        K.junk = T("junk", [128, 1024], F32)
        K.stat = Ring([T("stat%d" % i, [128, 8], F32) for i in range(4)])
        K.gates_all = T("gates_all", [128, 32, 4], F32); K.dst_all = T("dst_all", [128, 128], U32)
        K.widx = T("widx", [128, NBLK * 8], U32); K.bgidx = T("bgidx", [128, NBLK], U32); K.bdidx = T("bdidx", [128, NBLK], U32)
        K.reg_w = nc.gpsimd.to_reg(NEXP * D - 1); K.reg_bd = nc.gpsimd.to_reg(NEXP - 1); K.reg_bg = nc.gpsimd.to_reg(NEXP * 128 - 1)
        zt = T("zt", [128, 4, 1024], BF16); dzt = Dep()
        phases = [("A", phase_A), ("MEM", phase_MEM), ("NA", phase_NA), ("FT", phase_FT), ("OUT", phase_OUT), ("MOE", phase_MOE), ("FINAL", phase_FINAL)]
        for name, fn in phases:
            npt = 4 if name in ("A", "MOE") else 2
            with ExitStack() as pst:
                K.pT = Ring([pst.enter_context(nc.psum_tensor("pT_%s%d" % (name, i), [128, 1024], BF16)) for i in range(npt)], psum=True)
                K.pF = Ring([pst.enter_context(nc.psum_tensor("pF_%s%d" % (name, i), [128, 512], F32)) for i in range(8 - npt)], psum=True)
                fn(K)
                P.barrier()
            if name == "MEM":
                P.op("pool", lambda e: e.memset(zt[:, :, :], 0.0), writes=[dzt])
                for zi in range(NBLK):
                    P.dma("pool", lambda e, zi=zi: e.dma_start(out=K.Xs_d[zi * 512:(zi + 1) * 512, :].rearrange("(t p) d -> p t d", p=128), in_=zt[:, :, :]),
                          reads=[dzt], adds=[K.dXs])
            if stop_after == name:
                break
        if DEBUG:
            for nm, til, dt_, w_ in (("dbg_dst", K.dst_all, U32, 128), ("dbg_widx", K.widx, U32, NBLK * 8), ("dbg_bdidx", K.bdidx, U32, NBLK), ("dbg_bgidx", K.bgidx, U32, NBLK)):
                dd = nc.dram_tensor(nm, [128, w_], dt_, kind="ExternalOutput").ap()
                P.dma("sp", lambda e, dd=dd, til=til: e.dma_start(out=dd[:, :], in_=til[:, :]), adds=[K.dout])
            dd = nc.dram_tensor("dbg_gates", [128, 128], F32, kind="ExternalOutput").ap()
            P.dma("sp", lambda e, dd=dd: e.dma_start(out=dd[:, :], in_=K.gates_all[:, :, :].rearrange("p a b -> p (a b)")), adds=[K.dout])
        if stop_after is not None and stop_after != "FINAL":
            P.dma("sp", lambda e: e.dma_start(out=K.out[0:128, :], in_=K.junk[:, :]), adds=[K.dout])
        P.drain("sp")
        K.ninst = P.ninst
    return nc, K


def _consts():
    bf = ml_dtypes.bfloat16
    c = np.arange(128, dtype=np.float64)
    ang = 2 * np.pi * np.outer(c, c) / 128.0
    C, S = np.cos(ang), np.sin(ang)
    cs4 = np.concatenate([C, -S, -S, -C], axis=1).astype(bf)
    s2 = np.arange(128, dtype=np.float64)[:, None, None]
    s1 = np.arange(64, dtype=np.float64)[None, :, None]
    k2 = np.arange(128, dtype=np.float64)[None, None, :]
    th = 2 * np.pi * k2 * (s1 + 64 * s2) / 8192.0
    et = np.stack([np.cos(th), np.sin(th)], axis=2)
    sign = np.where(np.arange(128) % 2 == 0, 1.0, -1.0)[None, None, None, :]
    etw = [et.astype(bf), (et * sign).astype(bf)]
    dm = []
    s1v = np.arange(64, dtype=np.float64)[:, None]
    for hf in range(2):
        k1 = (hf * 32 + np.arange(32, dtype=np.float64))[None, :]
        ph = 2 * np.pi * k1 * s1v / 64.0
        dm.append((np.concatenate([np.cos(ph), np.sin(ph)], axis=0) * 2.0 ** -10).astype(bf))
    ident = np.eye(128).astype(bf)
    lo = np.stack([np.triu(np.ones((128, 128)), 1), np.ones((128, 128))], axis=1).astype(bf)
    iota = np.broadcast_to(np.arange(NEXP, dtype=np.float32)[None, :], (128, NEXP)).copy()
    cq = np.arange(64)
    col_start = np.clip(cq - 8, 0, 64 - 16)
    col_in = (cq[None, :] >= col_start[:, None]) & (cq[None, :] < col_start[:, None] + 16)
    mask = np.broadcast_to(col_in.T[None, :, None, None, :], (2, 64, 8, 4, 64)).reshape(128, 2048).astype(bf)
    rowoff = (np.arange(8, dtype=np.float32)[None, :] * 128 + np.arange(128, dtype=np.float32)[:, None]).astype(np.float32)
    return dict(cs4=cs4, etw=etw, dmat=dm, ident=ident, lo=lo, iota=iota, na_mask=mask, rowoff=rowoff)


def _kv_rows(hf):
    r0 = 64 * hf - 4
    rows = []
    for j in range(KVROWS):
        r = r0 + j
        if r < 0:
            r = 4 + j
        if r > 127:
            r = 120 + (r - 128)
        rows.append(r)
    return np.array(rows)


def _na_bias_table(rel_bias, hf):
    rows = _kv_rows(hf)
    ck = np.arange(64)[:, None]; cq = np.arange(64)[None, :]
    dc = np.clip(ck - cq, -15, 15) + 15
    out = np.empty((64, 64, 8, 8, 64), np.float32)
    for lq in range(64):
        r = 64 * hf + lq
        for j in range(8):
            dr = int(rows[lq + j] - r)
            assert -7 <= dr <= 7
            out[lq, :, :, j, :] = rel_bias[:, dr + 7, :][:, dc].transpose(1, 0, 2)
    sel = [lq for lq in range(64) if (lq <= 4 or lq >= 60)]
    o2 = out[sel][:, :, HORD].reshape(len(sel), 64, 8, 4, 2, 64).transpose(0, 4, 1, 2, 3, 5)
    return np.ascontiguousarray(o2).reshape(len(sel), 128, 2048)


def make_in_maps(inputs):
    f = lambda a: np.ascontiguousarray(np.asarray(a, dtype=np.float32))
    x = f(inputs["x"]); mem = f(inputs["mem"])
    cst = _consts()
    bc = lambda v: np.ascontiguousarray(np.broadcast_to(f(v).reshape(1, -1), (128, f(v).size)))
    shared = dict(
        gmix=bc(inputs["g_mix"][0]), gmem=bc(inputs["g_mem"][0]), ggrp=bc(inputs["g_grp"][0]), gffn=bc(inputs["g_ffn"][0]),
        gfin=bc(inputs["g_final"]), router_b=bc(inputs["router_b"][0]),
        w_in=f(inputs["w_in"][0]), w_kv=f(inputs["w_mem_kv"][0]), w_out=f(inputs["w_out"][0]), router_w=f(inputs["router_w"][0]),
        w_gu=f(inputs["w_gu"][0]).reshape(NEXP * D, 2048), w_down=f(inputs["w_down"][0]).reshape(NEXP * D, D),
        b_gu=np.ascontiguousarray(f(inputs["b_gu"][0]).reshape(NEXP, 16, 128).transpose(0, 2, 1)).reshape(NEXP * 128, 16),
        b_down=f(inputs["b_down"][0]), rowoff=cst["rowoff"],
        na_mask=cst["na_mask"], cs4=cst["cs4"], ident=cst["ident"], lo=cst["lo"], iota=cst["iota"],
    )
    rel = f(inputs["na_rel_bias"][0])
    tabs = [_na_bias_table(rel, hf) for hf in range(2)]
    maps = []
    for c in range(NCORES):
        b, hf = c // 2, c % 2
        xb = x[b]
        xc = np.concatenate([xb[hf * TOK:(hf + 1) * TOK], xb[(1 - hf) * TOK:(2 - hf) * TOK]], axis=0)
        rows = _kv_rows(hf)
        xkv = xb.reshape(128, 64, D)[rows].reshape(KVTOK, D)
        m = dict(shared)
        m.update(x_core=np.ascontiguousarray(xc), x_kv=np.ascontiguousarray(xkv), mem=mem[b], na_bias=tabs[hf],
                 etw=cst["etw"][hf], dmat=cst["dmat"][hf])
        maps.append(m)
    return maps


def kernel(**inputs):
    nc, K = build()
    maps = make_in_maps(inputs)
    res = run_bass_kernel_spmd(nc, maps, core_ids=list(range(NCORES)))
    out = np.empty((4, SEQ, D), np.float32)
    for c in range(NCORES):
        b, hf = c // 2, c % 2
        out[b, hf * TOK:(hf + 1) * TOK] = res.results[c]["out"]
    return out
```
